# Optimizing a Trainium2 kernel written in Bass

```python
import jax, jax.numpy as jnp
from jax import lax
import numpy as np

D_MODEL = 2048
BATCH = 4
SEQ = 2048
DEPTH = 4

POOL_WINDOWS = (2, 4, 8, 16)
POOL_GROUPS = len(POOL_WINDOWS)
POOL_GROUP_DIM = D_MODEL // 8
POOL_WIDTH = POOL_GROUPS * POOL_GROUP_DIM
FOX_HEAD_DIM = 128
FOX_HEADS = D_MODEL // (2 * FOX_HEAD_DIM)
FOX_WIDTH = FOX_HEADS * FOX_HEAD_DIM
RET_HEAD_DIM = 128
RET_HEADS = D_MODEL // (2 * RET_HEAD_DIM)
RET_WIDTH = RET_HEADS * RET_HEAD_DIM
ROPE_BASE = 10000.0
N_BRANCHES = 3
D_FF = ((8 * D_MODEL // 3 + 255) // 256) * 256
BLOCK = 128
RMS_EPS = 1e-6
GN_EPS = 1e-5
FORGET_BIAS_MEAN = 2.0

IN_SPLITS = (POOL_WIDTH,
             FOX_WIDTH, FOX_WIDTH, FOX_WIDTH,
             FOX_HEADS,
             RET_WIDTH, RET_WIDTH, RET_WIDTH,
             RET_WIDTH,
             N_BRANCHES * D_MODEL)
IN_WIDTH = sum(IN_SPLITS)
IN_INDICES = tuple(int(i) for i in np.cumsum(IN_SPLITS)[:-1])

kernel_name = "hybrid_pool_fox_retention_macaron"

F32 = jnp.float32


def rms_norm(x, g):
    xf = x.astype(F32)
    y = xf * lax.rsqrt(jnp.mean(xf * xf, axis=-1, keepdims=True) + RMS_EPS) * g.astype(F32)
    return y.astype(x.dtype)


def swiglu(h, w13, w2):
    a, b = jnp.split(h @ w13, 2, axis=-1)
    return (jax.nn.silu(a) * b) @ w2


def rotary(x, pos):
    half = x.shape[-1] // 2
    inv_freq = ROPE_BASE ** (-jnp.arange(half, dtype=F32) / half)
    ang = pos.astype(F32)[:, None] * inv_freq[None, :]
    cos = jnp.cos(ang)[None, :, None, :]
    sin = jnp.sin(ang)[None, :, None, :]
    xf = x.astype(F32)
    x1, x2 = xf[..., :half], xf[..., half:]
    return jnp.concatenate([x1 * cos - x2 * sin, x1 * sin + x2 * cos], axis=-1).astype(x.dtype)


def pool_mixer(u, w_pool, scale):
    b, s, _ = u.shape
    ug = u.reshape(b, s, POOL_GROUPS, POOL_GROUP_DIM).astype(F32)
    csum = jnp.cumsum(ug, axis=1)
    t = jnp.arange(s)
    means = []
    for g, w in enumerate(POOL_WINDOWS):
        cg = jnp.pad(csum[:, :, g], ((0, 0), (w, 0), (0, 0)))
        win_sum = cg[:, w:] - cg[:, :s]
        cnt = jnp.minimum(t + 1, w).astype(F32)[None, :, None]
        means.append(win_sum / cnt)
    pooled = jnp.stack(means, axis=2) - ug
    mixed = jnp.einsum('bsgi,gio->bsgo', pooled, w_pool.astype(F32))
    out = mixed.reshape(b, s, POOL_WIDTH) * scale.astype(F32)
    return out.astype(u.dtype)


def forgetting_attention(q, k, v, f_logit, f_bias):
    b, s, h, dh = q.shape
    log_f = jax.nn.log_sigmoid(f_logit.astype(F32) + f_bias.astype(F32))
    cum_f = jnp.cumsum(log_f, axis=1).transpose(0, 2, 1)
    qh, kh, vh = (a.transpose(0, 2, 1, 3) for a in (q, k, v))
    scale = dh ** -0.5
    local = jnp.arange(BLOCK)
    causal = local[:, None] >= local[None, :]
    outs = []
    for i in range(s // BLOCK):
        q0, q1 = i * BLOCK, (i + 1) * BLOCK
        kb, vb = kh[:, :, :q1], vh[:, :, :q1]
        logits = jnp.einsum('bhqd,bhkd->bhqk', qh[:, :, q0:q1], kb,
                            preferred_element_type=F32) * scale
        logits = logits + cum_f[:, :, q0:q1, None] - cum_f[:, :, None, :q1]
        mask = jnp.concatenate([jnp.ones((BLOCK, q0), dtype=bool), causal], axis=1)
        logits = jnp.where(mask[None, None], logits, -jnp.inf)
        p = jax.nn.softmax(logits, axis=-1)
        outs.append(jnp.einsum('bhqk,bhkd->bqhd', p.astype(vb.dtype), vb))
    return jnp.concatenate(outs, axis=1).reshape(b, s, h * dh)


def retention(q, k, v, gate):
    b, s, h, dh = q.shape
    n = s // BLOCK
    log_gamma = jnp.log1p(-jnp.power(2.0, -5.0 - jnp.arange(h, dtype=F32)))
    pos = jnp.arange(BLOCK, dtype=F32)
    diff = pos[:, None] - pos[None, :]
    inner_decay = jnp.where(diff[None] >= 0,
                            jnp.exp(jnp.maximum(diff, 0.0)[None] * log_gamma[:, None, None]),
                            0.0)
    xi = jnp.exp((pos + 1.0)[None, :] * log_gamma[:, None])
    zeta = jnp.exp((BLOCK - 1.0 - pos)[None, :] * log_gamma[:, None])
    chunk_decay = jnp.exp(BLOCK * log_gamma)

    qc = q.astype(F32).reshape(b, n, BLOCK, h, dh)
    kc = k.astype(F32).reshape(b, n, BLOCK, h, dh) * (dh ** -0.5)
    vc = v.astype(F32).reshape(b, n, BLOCK, h, dh)

    scores = jnp.einsum('bnqhd,bnkhd->bnhqk', qc, kc) * inner_decay[None, None]
    o_inner = jnp.einsum('bnhqk,bnkhe->bnqhe', scores, vc)

    kv = jnp.einsum('bnkhd,hk,bnkhe->bnhde', kc, zeta, vc)

    def step(state, kv_chunk):
        return chunk_decay[None, :, None, None] * state + kv_chunk, state

    _, prev = lax.scan(step, jnp.zeros((b, h, dh, dh), F32), kv.transpose(1, 0, 2, 3, 4))
    prev = prev.transpose(1, 0, 2, 3, 4)
    o_cross = jnp.einsum('bnqhd,hq,bnhde->bnqhe', qc, xi, prev)

    o = (o_inner + o_cross).reshape(b, s, h, dh)
    mu = jnp.mean(o, axis=-1, keepdims=True)
    var = jnp.mean(jnp.square(o - mu), axis=-1, keepdims=True)
    o = ((o - mu) * lax.rsqrt(var + GN_EPS)).reshape(b, s, h * dh)
    return (jax.nn.silu(gate.astype(F32)) * o).astype(q.dtype)


def hybrid_mixer(hn, w_in, forget_bias, pool_w, pool_scale,
                 w_branch_pool, w_branch_fox, w_branch_ret, w_out):
    b, s, d = hn.shape
    z = hn @ w_in
    (u_pool, fq, fk, fv, f_logit, rq, rk, rv, rgate, gate_logits) = jnp.split(z, IN_INDICES, axis=-1)
    pos = jnp.arange(s)

    y_pool = pool_mixer(u_pool, pool_w, pool_scale)

    hsplit = lambda a, nh, hd: a.reshape(b, s, nh, hd)
    y_fox = forgetting_attention(hsplit(fq, FOX_HEADS, FOX_HEAD_DIM),
                                 hsplit(fk, FOX_HEADS, FOX_HEAD_DIM),
                                 hsplit(fv, FOX_HEADS, FOX_HEAD_DIM),
                                 f_logit, forget_bias)

    y_ret = retention(rotary(hsplit(rq, RET_HEADS, RET_HEAD_DIM), pos),
                      rotary(hsplit(rk, RET_HEADS, RET_HEAD_DIM), pos),
                      hsplit(rv, RET_HEADS, RET_HEAD_DIM), rgate)

    gates = jax.nn.sigmoid(gate_logits.astype(F32)).reshape(b, s, N_BRANCHES, d)
    merged = (gates[:, :, 0] * (y_pool @ w_branch_pool).astype(F32)
              + gates[:, :, 1] * (y_fox @ w_branch_fox).astype(F32)
              + gates[:, :, 2] * (y_ret @ w_branch_ret).astype(F32))
    return merged.astype(hn.dtype) @ w_out


def setup_inputs(seed: int = 0) -> dict:
    key = jax.random.key(seed)
    ks = jax.random.split(key, 20)

    def nrm(k, shape, fan_in):
        return jax.random.normal(k, shape, F32) * (fan_in ** -0.5)

    def gain(k, shape):
        return 1.0 + 0.05 * jax.random.normal(k, shape, F32)

    return {
        "x": jax.random.normal(ks[0], (BATCH, SEQ, D_MODEL), F32),
        "ffn1_norm": gain(ks[1], (DEPTH, D_MODEL)),
        "ffn1_w13": nrm(ks[2], (DEPTH, D_MODEL, 2 * D_FF), D_MODEL),
        "ffn1_w2": nrm(ks[3], (DEPTH, D_FF, D_MODEL), D_FF),
        "mix_norm": gain(ks[4], (DEPTH, D_MODEL)),
        "w_in": nrm(ks[5], (DEPTH, D_MODEL, IN_WIDTH), D_MODEL),
        "forget_bias": FORGET_BIAS_MEAN + 0.5 * jax.random.normal(ks[6], (DEPTH, FOX_HEADS), F32),
        "pool_w": nrm(ks[7], (DEPTH, POOL_GROUPS, POOL_GROUP_DIM, POOL_GROUP_DIM), POOL_GROUP_DIM),
        "pool_scale": 1.0 + 0.1 * jax.random.normal(ks[8], (DEPTH, POOL_WIDTH), F32),
        "w_branch_pool": nrm(ks[9], (DEPTH, POOL_WIDTH, D_MODEL), POOL_WIDTH),
        "w_branch_fox": nrm(ks[10], (DEPTH, FOX_WIDTH, D_MODEL), FOX_WIDTH),
        "w_branch_ret": nrm(ks[11], (DEPTH, RET_WIDTH, D_MODEL), RET_WIDTH),
        "w_out": nrm(ks[12], (DEPTH, D_MODEL, D_MODEL), D_MODEL),
        "ffn2_norm": gain(ks[13], (DEPTH, D_MODEL)),
        "ffn2_w13": nrm(ks[14], (DEPTH, D_MODEL, 2 * D_FF), D_MODEL),
        "ffn2_w2": nrm(ks[15], (DEPTH, D_FF, D_MODEL), D_FF),
        "final_norm": gain(ks[16], (D_MODEL,)),
    }


def reference(x, ffn1_norm, ffn1_w13, ffn1_w2, mix_norm, w_in, forget_bias, pool_w, pool_scale,
              w_branch_pool, w_branch_fox, w_branch_ret, w_out, ffn2_norm, ffn2_w13, ffn2_w2,
              final_norm):
    for l in range(DEPTH):
        x = x + 0.5 * swiglu(rms_norm(x, ffn1_norm[l]), ffn1_w13[l], ffn1_w2[l])
        x = x + hybrid_mixer(rms_norm(x, mix_norm[l]), w_in[l], forget_bias[l], pool_w[l],
                             pool_scale[l], w_branch_pool[l], w_branch_fox[l], w_branch_ret[l],
                             w_out[l])
        x = x + 0.5 * swiglu(rms_norm(x, ffn2_norm[l]), ffn2_w13[l], ffn2_w2[l])
    return rms_norm(x, final_norm)
```

```python
import os
import numpy as np
import ml_dtypes
import concourse.bass as bass
import concourse.mybir as mybir
from concourse.bass_utils import run_bass_kernel_spmd

F32 = mybir.dt.float32
BF16 = mybir.dt.bfloat16
ALU = mybir.AluOpType
AF = mybir.ActivationFunctionType

D = 2048
S = 2048
KC = 16
TT = 1024
NT = S // TT
SUB = 512
NS = TT // SUB
NQ = S // SUB
DFF = 5632
FC = 44
H = 8
NB = S // 128
DEPTH = 4
RMS_EPS = 1e-6
GN_EPS = 1e-5
OFF_POOL, OFF_FQ, OFF_FK, OFF_FV, OFF_FL, OFF_RQ, OFF_RK, OFF_RV, OFF_RG, OFF_GATE = (
    0, 1024, 2048, 3072, 4096, 4104, 5128, 6152, 7176, 8200)
NSLOT = 5
STRICT = int(os.environ.get("MK_STRICT", "1"))
SLOT_ELEMS = 4096


class Op:
    __slots__ = ("eng", "fn", "deps", "dsem", "signal", "sem", "inc", "waits", "val")

    def __init__(self, eng, fn, deps, dsem):
        self.eng, self.fn, self.deps, self.dsem = eng, fn, deps, dsem
        self.signal = dsem is not None
        self.sem = None
        self.inc = 0
        self.val = 0
        self.waits = []


class Prog:
    ENGS = ("pe", "act", "dve", "pool", "sp")

    def __init__(self, nc):
        self.nc = nc
        self.ops = []
        self.lw = {}
        self.rd = {}
        self.dry = False
        self.last_eng = {}
        self.last_dma = {}

    def op(self, eng, fn, reads=(), writes=(), dsem=None):
        if self.dry:
            return
        i = len(self.ops)
        deps = set()
        for k in reads:
            j = self.lw.get(k)
            if j is not None:
                deps.add(j)
        for k in writes:
            j = self.lw.get(k)
            if j is not None:
                deps.add(j)
            deps.update(self.rd.get(k, ()))
        for k in reads:
            self.rd.setdefault(k, []).append(i)
        for k in writes:
            self.lw[k] = i
            self.rd[k] = []
        self.ops.append(Op(eng, fn, deps, dsem))
        if dsem is None:
            self.last_eng[eng] = i
        else:
            self.last_dma[dsem] = i

    def barrier(self):
        if self.dry:
            return
        deps = set(self.last_eng.values()) | set(self.last_dma.values())
        for e in self.ENGS:
            self.ops.append(Op(e, None, set(deps), None))
        self.lw = {}
        self.rd = {}

    def finalize(self):
        nc = self.nc
        ops = self.ops
        for o in ops:
            for j in o.deps:
                d = ops[j]
                if d.dsem is None and (STRICT or d.eng != o.eng):
                    d.signal = True
        esem = {e: nc.alloc_semaphore("s_" + e) for e in self.ENGS}
        dsems = {}
        ecnt = {e: 0 for e in self.ENGS}
        dcnt = {}
        for o in ops:
            if o.dsem is not None:
                if o.dsem not in dsems:
                    dsems[o.dsem] = nc.alloc_semaphore("d_" + o.dsem)
                    dcnt[o.dsem] = 0
                dcnt[o.dsem] += 16
                o.sem, o.inc, o.val = dsems[o.dsem], 16, dcnt[o.dsem]
            elif o.signal:
                ecnt[o.eng] += 1
                o.sem, o.inc, o.val = esem[o.eng], 1, ecnt[o.eng]
        waited = {e: {} for e in self.ENGS}
        for o in ops:
            need = {}
            for j in o.deps:
                d = ops[j]
                if d.dsem is None and d.eng == o.eng and (not STRICT or o.eng == "pe"):
                    continue
                key = id(d.sem)
                if key not in need or need[key][1] < d.val:
                    need[key] = (d.sem, d.val)
            w = waited[o.eng]
            for key, (sem, val) in need.items():
                if w.get(key, 0) >= val:
                    continue
                w[key] = val
                o.waits.append((sem, val))
        self.by_eng = {e: [o for o in ops if o.eng == e] for e in self.ENGS}
        if os.environ.get("MK_VERBOSE"):
            print("sem counts", ecnt, "dma", {k: v for k, v in dcnt.items()}, "nops", len(ops), flush=True)

    def emit(self):
        nc = self.nc
        self.finalize()

        def mk(name):
            def body(e):
                for o in self.by_eng[name]:
                    for sem, val in o.waits:
                        e.wait_ge(sem, val)
                    if o.fn is not None:
                        ins = o.fn(e)
                        if o.signal:
                            ins.then_inc(o.sem, o.inc)
            return body

        with nc.Block() as block:
            block.tensor(mk("pe"))
            block.scalar(mk("act"))
            block.vector(mk("dve"))
            block.gpsimd(mk("pool"))
            block.sync(mk("sp"))


def log_gamma(h):
    return float(np.log1p(-np.power(2.0, -5.0 - h)))


def build(nlayers):
    nc = bass.Bass("TRN2", target_bir_lowering=False)
    P = Prog(nc)
    L = nlayers

    def din(name, shape, dt=F32):
        return nc.dram_tensor(name, shape, dt, kind="ExternalInput").ap()

    xin = din("xin", [128, KC, S])
    w13d = din("w13", [L * 2 * FC * 128, 4096])
    w2d = din("w2", [L * 2 * 32 * 128, 2816])
    wind = din("win", [L * 32 * 128, 4096])
    wfld = din("wfl", [L * 128, KC * 72])
    wgad = din("wga", [L * 16 * 128, 4096])
    wgbd = din("wgb", [L * 16 * 128, 2048])
    wbrd = din("wbr", [L * 16 * 128, 3072])
    woutd = din("wout", [L * 8 * 128, 4096])
    pwd = din("pw", [L * 4 * 128, 512])
    NG = L * 3 + 1
    NCF = NG * 16 + L * 8 + L + 8 + 8 + 8 * 128 + 8 * 128 + 64
    cfd = din("cf", [128, NCF])
    NCB = 4 * 128 + 8 * 128
    cbd = din("cb", [128, NCB], BF16)
    cosd = din("cosr", [128, NB, 256])
    sind = din("sinr", [128, NB, 256])
    xout = nc.dram_tensor("xout", [128, KC, S], F32, kind="ExternalOutput").ap()
    yout = nc.dram_tensor("yout", [128, KC, S], F32, kind="ExternalOutput").ap()

    DBG = os.environ.get("MK_DEBUG") == "1"

    def dscr(name, shape, dt=BF16):
        if DBG and name != "XS":
            return nc.dram_tensor(name, shape, dt, kind="ExternalOutput").ap()
        return nc.dram_tensor(name, shape, dt).ap()

    XS = dscr("XS", [128, KC, S], F32)
    XNS = dscr("XNS", [128, KC, S])
    US = dscr("US", [128, 8, S])
    FQS = dscr("FQS", [128, 8, S])
    FKS = dscr("FKS", [128, 8, S])
    RGS = dscr("RGS", [128, 8, S])
    FVS = dscr("FVS", [128, NB, 1024])
    RVS = dscr("RVS", [128, NB, 1024])
    RQS = dscr("RQS", [128, NB, 1024])
    RKS = dscr("RKS", [128, NB, 1024])
    YPS = dscr("YPS", [128, 8, S])
    YFS = dscr("YFS", [128, 8, S])
    YRS = dscr("YRS", [128, 8, S])

    base = (nc.sbuf_base + 63) // 64 * 64
    top = nc.sbuf_top
    cur = [base]

    uid = [0]
    offs = {}
    limit = [top]
    GS_OFF = (top - 8192 - 4096 - 128) // 64 * 64

    def alloc(name, shape, dt, at=None):
        uid[0] += 1
        name = f"{name}_{uid[0]}"
        nbytes = int(np.prod(shape[1:])) * (4 if dt == F32 else 2)
        if at is None:
            off = cur[0]
            cur[0] = (off + nbytes + 63) // 64 * 64
            assert off + nbytes <= limit[0], (name, off, nbytes, limit[0])
        else:
            off = at
        assert off + nbytes <= top, (name, off, nbytes, top)
        offs[name.rsplit("_", 1)[0]] = off
        return nc.alloc_sbuf_tensor_at(name, list(shape), dt, offset=off)

    WSL = [alloc(f"ws{i}", [128, SLOT_ELEMS], BF16) for i in range(NSLOT)]
    XT = [alloc("xt0", [128, KC, TT], F32)]
    XN = alloc("xn", [128, KC, TT], BF16)
    CF = alloc("cf", [128, NCF], F32)
    CB = alloc("cb", [128, NCB], BF16)
    NFB = alloc("nfb", [128, L], F32)
    SQ = [alloc(f"sq{i}", [128, SUB], BF16) for i in range(2)]
    RMS = alloc("rms", [128, SUB], F32)
    RSTD = alloc("rstd", [128, SUB], F32)
    ov_base = cur[0]

    o = 0
    GAIN = CF[:, o:o + NG * 16].rearrange("p (g k) -> p g k", k=16); o += NG * 16
    PSC = CF[:, o:o + L * 8].rearrange("p (l c) -> p l c", c=8); o += L * 8
    FBR = CF[:, o:o + L]; o += L
    ZETA = CF[:, o:o + 8]; o += 8
    I8 = CF[:, o:o + 8]; o += 8
    DTAB = CF[:, o:o + 1024].rearrange("p (h q) -> p h q", h=8); o += 1024
    XIROW = CF[:, o:o + 1024].rearrange("p (h q) -> p h q", h=8); o += 1024
    INVC = CF[:, o:o + 64].rearrange("p (g j) -> p g j", g=4); o += 64
    ONESB = CB[:, 0:128]
    ONES128 = CB[:, 128:256]
    IDENT = CB[:, 256:384]
    NEGM = CB[:, 384:512]
    SEL = CB[:, 512:512 + 1024].rearrange("p (h m) -> p h m", h=8)

    PS = [nc.alloc_psum_tensor(f"ps{i}", [128, 512], F32) for i in range(6)]
    psi = [0]
    pti = [0]

    def next_ps():
        i = psi[0] % 6
        psi[0] += 1
        return i

    def next_pt():
        i = 2 + pti[0] % 4
        pti[0] += 1
        return i

    class WStream:
        def __init__(self):
            self.reqs = []
            self.cons = 0
            self.issued = 0
            self.safe = 0

        def get(self, src, n, group=False):
            j = self.cons
            self.cons += 1
            if not group:
                self.safe = j
            if P.dry:
                self.reqs.append((src, n))
            else:
                while self.issued < min(len(self.reqs), j + NSLOT) and self.issued - NSLOT < self.safe:
                    i = self.issued
                    s = i % NSLOT
                    src_i, n_i = self.reqs[i]
                    P.op("pool", lambda e, s=s, src_i=src_i, n_i=n_i: e.dma_start(out=WSL[s][:, 0:n_i], in_=src_i),
                         reads=(), writes=[("ws", s)], dsem=f"ws{s}")
                    self.issued += 1
            return WSL[j % NSLOT], ("ws", j % NSLOT)

    W = WStream()

    def mm(out, pairs, reads, writes, start=True, stop=True):
        def fn(e, out=out, pairs=pairs, start=start, stop=stop):
            n = len(pairs)
            ins = None
            for i, (l, r) in enumerate(pairs):
                ins = e.matmul(out, l, r, start=(start and i == 0), stop=(stop and i == n - 1))
            return ins
        P.op("pe", fn, reads, writes)

    def act(out, in_, func, reads, writes, bias=None, scale=None):
        def fn(e):
            kw = {}
            if bias is not None:
                kw["bias"] = bias
            if scale is not None:
                kw["scale"] = scale
            return e.activation(out=out, in_=in_, func=func, **kw)
        P.op("act", fn, reads, writes)

    def tt(out, a, b, op, reads, writes, eng="dve"):
        P.op(eng, lambda e: e.tensor_tensor(out=out, in0=a, in1=b, op=op), reads, writes)

    def stt(out, a, sc, b, op0, op1, reads, writes, eng="dve"):
        P.op(eng, lambda e: e.scalar_tensor_tensor(out=out, in0=a, scalar=sc, in1=b, op0=op0, op1=op1), reads, writes)

    def ts(out, a, s1, op0, reads, writes, s2=None, op1=None, eng="dve"):
        def fn(e):
            if op1 is None:
                return e.tensor_scalar(out=out, in0=a, scalar1=s1, scalar2=None, op0=op0)
            return e.tensor_scalar(out=out, in0=a, scalar1=s1, scalar2=s2, op0=op0, op1=op1)
        P.op(eng, fn, reads, writes)

    def cp(out, in_, reads, writes, eng="dve"):
        P.op(eng, lambda e: e.tensor_copy(out=out, in_=in_), reads, writes)

    def recip(out, in_, reads, writes):
        P.op("dve", lambda e: e.reciprocal(out=out, in_=in_), reads, writes)

    def memset(ap, v, writes, eng="dve"):
        P.op(eng, lambda e: e.memset(ap, v), (), writes)

    def dma(out, in_, reads, writes, dsem, eng="sp"):
        P.op(eng, lambda e: e.dma_start(out=out, in_=in_), reads, writes, dsem=dsem)

    xtk = [("xt", 0, k, sb) for k in range(KC) for sb in range(NS)]
    xnk_all = [("xn", k, sb) for k in range(KC) for sb in range(NS)]

    def xnk(sub):
        return [("xn", k, sub) for k in range(KC)]

    def ssl(sub):
        return slice(sub * SUB, (sub + 1) * SUB)

    def norm_sub(gi, sub, out_fn, out_keys_fn):
        pss = next_ps()
        for kc in range(KC):
            act(SQ[kc % 2][:], XT[0][:, kc, ssl(sub)], AF.Square, [("xt", 0, kc, sub)], [("sq", kc % 2)])
            mm(PS[pss][:], [(ONESB, SQ[kc % 2][:])], [("sq", kc % 2)], [("ps", pss)], start=(kc == 0), stop=(kc == KC - 1))
        act(RMS[:], PS[pss][:], AF.Sqrt, [("ps", pss)], ["rms"], bias=EPS_RMS[:, 0:1], scale=1.0 / D)
        recip(RSTD[:], RMS[:], ["rms"], ["rstd"])
        for kc in range(KC):
            stt(out_fn(kc), XT[0][:, kc, ssl(sub)], GAIN[:, gi, kc:kc + 1], RSTD[:], ALU.mult, ALU.mult,
                [("xt", 0, kc, sub), "rstd"], out_keys_fn(kc))

    def norm_xn(gi):
        for sub in range(NS):
            norm_sub(gi, sub, lambda kc, sub=sub: XN[:, kc, ssl(sub)], lambda kc, sub=sub: [("xn", kc, sub)])

    EPS_RMS = alloc("epsr", [128, 2], F32)

    def ffn_stage(l, which, src, dst, final):
        off = cur[0]
        limit[0] = top
        GT = alloc("gT", [128, 22, TT], BF16)
        SA = [alloc(f"sa{i}", [128, SUB], F32) for i in range(2)]
        cur[0] = off
        gi = l * 3 + (0 if which == 0 else 2)
        si = 0
        for t in range(NT):
            tsl = slice(t * TT, (t + 1) * TT)
            dma(XT[0][:], src[:, :, tsl], (), xtk, "xld0")
            norm_xn(gi)
            for hh in range(2):
                for f in range(22):
                    fg = hh * 22 + f
                    r0 = ((l * 2 + which) * FC + fg) * 128
                    w, wk = W.get(w13d[r0:r0 + 128, :], 4096)
                    w3 = w[:, 0:4096].rearrange("p (k c) -> p k c", k=KC)
                    for sub in range(NS):
                        pa, pb = next_ps(), next_ps()
                        mm(PS[pa][:], [(w3[:, kc, 0:128], XN[:, kc, ssl(sub)]) for kc in range(KC)], [wk] + xnk(sub), [("ps", pa)])
                        mm(PS[pb][:], [(w3[:, kc, 128:256], XN[:, kc, ssl(sub)]) for kc in range(KC)], [wk] + xnk(sub), [("ps", pb)])
                        r = si % 2
                        si += 1
                        act(SA[r][:], PS[pa][:], AF.Silu, [("ps", pa)], [("sa", r)])
                        tt(GT[:, f, ssl(sub)], SA[r][:], PS[pb][:], ALU.mult, [("sa", r), ("ps", pb)], [("gT", f, sub)])
                for dc in range(KC):
                    r0 = (((l * 2 + which) * 16 + dc) * 2 + hh) * 128
                    w, wk = W.get(w2d[r0:r0 + 128, :], 2816)
                    w3 = w[:, 0:2816].rearrange("p (f c) -> p f c", f=22)
                    for sub in range(NS):
                        py = next_ps()
                        mm(PS[py][:], [(w3[:, fc, :], GT[:, fc, ssl(sub)]) for fc in range(22)],
                           [wk] + [("gT", fc, sub) for fc in range(22)], [("ps", py)])
                        stt(XT[0][:, dc, ssl(sub)], PS[py][:], 0.5, XT[0][:, dc, ssl(sub)], ALU.mult, ALU.add,
                            [("ps", py), ("xt", 0, dc, sub)], [("xt", 0, dc, sub)])
            dma(dst[:, :, tsl], XT[0][:], xtk, (), "xst0")
            if final:
                P.barrier()
                YO = alloc("yo", [128, KC, SUB], F32, at=off)
                for sub in range(NS):
                    norm_sub(L * 3, sub, lambda kc: YO[:, kc, :], lambda kc: [("yo", kc)])
                    dma(yout[:, :, t * TT + sub * SUB:t * TT + (sub + 1) * SUB], YO[:], [("yo", k) for k in range(KC)], (), "yo")
                P.barrier()
        P.barrier()

    def m1_stage(l):
        off = cur[0]
        limit[0] = GS_OFF
        NTB = TT // 128
        STF = [alloc(f"stf{i}", [128, 2, TT], BF16) for i in range(2)]
        STT = [alloc(f"stt{i}", [128, NTB, 256], BF16) for i in range(2)]
        RT = [alloc(f"rt{i}", [128, 256], F32) for i in range(2)]
        RU = [alloc(f"ru{i}", [128, 256], F32) for i in range(2)]
        COST = alloc("cost", [128, NTB, 256], F32)
        SINT = alloc("sint", [128, NTB, 256], F32)
        ETMP = alloc("etmp", [128, SUB], F32)
        cur[0] = off
        gi = l * 3 + 1
        fi = [0]
        ti = [0]
        ri = [0]
        fams_f = [(OFF_POOL, US, "pool"), (OFF_FQ, FQS, "fq"), (OFF_FK, FKS, "fk"), (OFF_RG, RGS, "rg")]
        fams_t = [(OFF_FV, FVS, "fv"), (OFF_RV, RVS, "rv"), (OFF_RQ, RQS, "rq"), (OFF_RK, RKS, "rk")]
        for t in range(NT):
            tsl = slice(t * TT, (t + 1) * TT)
            dma(XT[0][:], XS[:, :, tsl], (), xtk, "xld0")
            dma(COST[:], cosd[:, t * NTB:(t + 1) * NTB, :], (), ["cost"], "cld1")
            dma(SINT[:], sind[:, t * NTB:(t + 1) * NTB, :], (), ["sint"], "cld2")
            norm_xn(gi)
            dma(XNS[:, :, tsl], XN[:], xnk_all, (), "xnst")
            for fidx, (coff, scr, nm) in enumerate(fams_f):
                for g in range(4):
                    r0 = ((l * 32) + fidx * 4 + g) * 128
                    w, wk = W.get(wind[r0:r0 + 128, :], 4096)
                    w3 = w[:, 0:4096].rearrange("p (k c) -> p k c", k=KC)
                    r = fi[0] % 2
                    fi[0] += 1
                    for j in range(2):
                        for sub in range(NS):
                            p = next_ps()
                            mm(PS[p][:], [(w3[:, kc, j * 128:(j + 1) * 128], XN[:, kc, ssl(sub)]) for kc in range(KC)],
                               [wk] + xnk(sub), [("ps", p)])
                            dst_ap = STF[r][:, j, ssl(sub)]
                            wkeys = [("stf", r, j, sub)]
                            if nm == "pool":
                                act(dst_ap, PS[p][:], AF.Copy, [("ps", p)], wkeys)
                            elif nm == "fq":
                                act(dst_ap, PS[p][:], AF.Copy, [("ps", p)], wkeys, scale=float(128 ** -0.5))
                            elif nm == "fk":
                                cp(dst_ap, PS[p][:], [("ps", p)], wkeys)
                            else:
                                act(dst_ap, PS[p][:], AF.Silu, [("ps", p)], wkeys)
                    dma(scr[:, g * 2:g * 2 + 2, tsl], STF[r][:], [("stf", r, j, sb) for j in range(2) for sb in range(NS)], (), f"stf{r}")
            for fidx, (coff, scr, nm) in enumerate(fams_t):
                for g in range(4):
                    r0 = ((l * 32) + 16 + fidx * 4 + g) * 128
                    w, wk = W.get(wind[r0:r0 + 128, :], 4096)
                    w3 = w[:, 0:4096].rearrange("p (k c) -> p k c", k=KC)
                    r = ti[0] % 2
                    ti[0] += 1
                    for tb in range(NTB):
                        p = next_ps()
                        mm(PS[p][:, 0:256], [(XN[:, kc, tb * 128:(tb + 1) * 128], w3[:, kc, :]) for kc in range(KC)],
                           [wk] + xnk(tb // 4), [("ps", p)])
                        if nm == "fv":
                            cp(STT[r][:, tb, :], PS[p][:, 0:256], [("ps", p)], [("stt", r, tb)])
                        elif nm == "rv":
                            for hh in range(2):
                                h = g * 2 + hh
                                act(STT[r][:, tb, hh * 128:(hh + 1) * 128], PS[p][:, hh * 128:(hh + 1) * 128], AF.Copy,
                                    [("ps", p)], [("stt", r, tb, hh)], scale=ZETA[:, h:h + 1])
                        else:
                            q = ri[0] % 2
                            ri[0] += 1
                            x4 = PS[p][:, 0:256].rearrange("p (h t d) -> p h t d", h=2, t=2)
                            s4 = SINT[:, tb, :].rearrange("p (h t d) -> p h t d", h=2, t=2)
                            u4 = RU[q][:].rearrange("p (h t d) -> p h t d", h=2, t=2)
                            tt(RT[q][:], PS[p][:, 0:256], COST[:, tb, :], ALU.mult, [("ps", p), "cost"], [("rt", q)])
                            tt(u4[:, :, 0, :], x4[:, :, 1, :], s4[:, :, 0, :], ALU.mult, [("ps", p), "sint"], [("ru", q, 0)])
                            tt(u4[:, :, 1, :], x4[:, :, 0, :], s4[:, :, 1, :], ALU.mult, [("ps", p), "sint"], [("ru", q, 1)])
                            tt(STT[r][:, tb, :], RT[q][:], RU[q][:], ALU.add, [("rt", q), ("ru", q, 0), ("ru", q, 1)],
                               [("stt", r, tb)])
                    rk = [("stt", r, tb) for tb in range(NTB)] + [("stt", r, tb, hh) for tb in range(NTB) for hh in range(2)]
                    dma(scr[:, t * NTB:(t + 1) * NTB, g * 256:(g + 1) * 256], STT[r][:], rk, (), f"stt{r}")
            w, wk = W.get(wfld[l * 128:(l + 1) * 128, :], KC * 72)
            w3 = w[:, 0:KC * 72].rearrange("p (k c) -> p k c", k=KC)
            for sub in range(NS):
                p = next_ps()
                mm(PS[p][0:72, :], [(w3[:, kc, :], XN[:, kc, ssl(sub)]) for kc in range(KC)], [wk] + xnk(sub), [("ps", p)])
                act(ETMP[0:72, :], PS[p][0:72, :], AF.Exp, [("ps", p)], ["etmp"], bias=NFB[0:72, l:l + 1], scale=-1.0)
                c0 = t * TT + sub * SUB
                act(GS[0:72, c0:c0 + SUB], ETMP[0:72, :], AF.Ln, ["etmp"], [("gs", t, sub)], bias=ONE_C[0:72, 0:1], scale=1.0)
        P.barrier()

    def m2_stage(l):
        off = cur[0]
        limit[0] = GS_OFF
        UB = alloc("ub", [128, S], BF16)
        A = alloc("pa", [128, S + 16], F32)
        S1 = alloc("ps1", [128, S + 16], F32)
        S2 = alloc("ps2", [128, S + 16], F32)
        PL = alloc("pl", [128, 2, S], BF16)
        T16 = alloc("t16", [128, 16], F32)
        YST = [alloc(f"yst{i}", [128, SUB], BF16) for i in range(2)]
        cur[0] = off
        memset(A[:, 0:16], 0.0, ["a"])
        memset(S1[:, 0:16], 0.0, ["s1"])
        memset(S2[:, 0:16], 0.0, ["s2"])
        yi = 0
        for g in range(4):
            wdw = 2 ** (g + 1)
            for ic in range(2):
                c = g * 2 + ic
                dma(UB[:], US[:, c, :], (), ["ub"], "uld")
                act(A[:, 16:], UB[:], AF.Copy, ["ub"], ["a"])
                curt, curk = A, "a"
                for k in range(g + 1):
                    sh = 2 ** k
                    dst, dk = (S1, "s1") if k % 2 == 0 else (S2, "s2")
                    tt(dst[:, 16:], curt[:, 16:], curt[:, 16 - sh:S + 16 - sh], ALU.add, [curk], [dk])
                    curt, curk = dst, dk
                stt(PL[:, ic, :], curt[:, 16:], 1.0 / wdw, A[:, 16:], ALU.mult, ALU.subtract, [curk, "a"], [("pl", ic)])
                tt(T16[:], curt[:, 16:32], INVC[:, g, :], ALU.mult, [curk], ["t16"])
                tt(PL[:, ic, 0:16], T16[:], A[:, 16:32], ALU.subtract, ["t16", "a", ("pl", ic)], [("pl", ic)])
            w, wk = W.get(pwd[(l * 4 + g) * 128:(l * 4 + g + 1) * 128, :], 512)
            w3 = w[:, 0:512].rearrange("p (i o) -> p i o", i=2)
            for oc in range(2):
                for th in range(NQ):
                    p = next_ps()
                    mm(PS[p][:], [(w3[:, ic, oc * 128:(oc + 1) * 128], PL[:, ic, th * SUB:(th + 1) * SUB]) for ic in range(2)],
                       [wk, ("pl", 0), ("pl", 1)], [("ps", p)])
                    r = yi % 2
                    yi += 1
                    act(YST[r][:], PS[p][:], AF.Copy, [("ps", p)], [("yst", r)], scale=PSC[:, l, g * 2 + oc:g * 2 + oc + 1])
                    dma(YPS[:, g * 2 + oc, th * SUB:(th + 1) * SUB], YST[r][:], [("yst", r)], (), f"yst{r}")
        P.barrier()

    def m3_stage(l):
        off = cur[0]
        limit[0] = GS_OFF
        GP = alloc("gp", [128, S], F32)
        R1 = alloc("r1", [128, S], F32)
        R2 = alloc("r2", [128, S], F32)
        MID = alloc("mid", [128, S], BF16)
        cur[0] = off
        src, sk, dst, dk = GS, "gs", GP, "gp"
        s = 1
        while s < S:
            tt(dst[0:72, s:], src[0:72, s:], src[0:72, 0:S - s], ALU.add, [sk], [dk])
            cp(dst[0:72, 0:s], src[0:72, 0:s], [sk, dk], [dk])
            src, sk, dst, dk = dst, dk, src, sk
            s *= 2
        G, gk = src, [sk]
        memset(FR[:], 0.0, ["fr"])
        cp(FR[0:72, :], G[0:72, :], gk + ["fr"], ["fr"])
        tt(R1[32:40, :], G[32:40, :], FR[32:40, :], ALU.subtract, gk + ["fr"], ["r1"])
        tt(R1[64:72, :], G[64:72, :], FR[64:72, :], ALU.subtract, gk + ["fr"], ["r1b"])
        cp(MID[32:40, :], R1[32:40, :], ["r1"], ["mid"])
        cp(MID[64:72, :], R1[64:72, :], ["r1b"], ["midb"])
        tt(R2[64:72, :], R1[64:72, :], MID[64:72, :], ALU.subtract, ["r1b", "midb"], ["r2"])
        cp(FR[32:40, :], MID[32:40, :], ["mid", "fr", "r1", "r1b"], ["fr"])
        cp(FR[64:72, :], R2[64:72, :], ["r2", "fr"], ["fr"])
        p = next_ps()
        for kb in range(NB):
            mm(PS[p][:, kb * 8:(kb + 1) * 8], [(G[0:8, kb * 128:(kb + 1) * 128], I8[0:8, 0:8])], gk, [("ps", p)])
        act(GTT[:], PS[p][:, 0:128], AF.Copy, [("ps", p)], ["gtt"])
        P.barrier()
        off = cur[0]
        FQHs = [alloc(f"fqh{i}", [128, S], BF16) for i in range(2)]
        FKHs = [alloc(f"fkh{i}", [128, S], BF16) for i in range(2)]
        FVHs = [alloc(f"fvh{i}", [128, NB, 128], BF16) for i in range(2)]
        PTL = [alloc(f"ptl{i}", [128, SUB], BF16) for i in range(3)]
        RDEN = alloc("rden", [128, SUB], F32)
        YST = [alloc(f"yst{i}", [128, SUB], BF16) for i in range(2)]
        cur[0] = off
        pi = 0
        yi = 0

        def head_loads(h):
            hb = h % 2
            dma(FQHs[hb][:], FQS[:, h, :], (), [("fqh", hb), "hchain"], "hld0")
            dma(FKHs[hb][:], FKS[:, h, :], (), [("fkh", hb), "hchain"], "hld0")
            dma(FVHs[hb][:], FVS[:, :, h * 128:(h + 1) * 128], (), [("fvh", hb), "hchain"], "hld0")

        head_loads(0)
        for h in range(H):
            hb = h % 2
            FQH, FKH, FVH = FQHs[hb], FKHs[hb], FVHs[hb]
            if h + 1 < H:
                head_loads(h + 1)
            for qg in range(NQ):
                po, pd = (2, 3) if (h * NQ + qg) % 2 == 0 else (4, 5)
                nkb = 4 * qg + 4
                for kb in range(nkb):
                    c0 = max(0, kb * 128 - qg * SUB)
                    n = SUB - c0
                    qs = slice(qg * SUB + c0, (qg + 1) * SUB)
                    diag = kb >= 4 * qg
                    psn = pi % 2

                    def fn(e, psn=psn, kb=kb, qs=qs, n=n, diag=diag, h=h, FKH=FKH, FQH=FQH):
                        e.matmul(PS[psn][:, 0:n], FKH[:, kb * 128:(kb + 1) * 128], FQH[:, qs], start=True, stop=False)
                        if diag:
                            e.matmul(PS[psn][:, 0:128], IDENT, NEGM, start=False, stop=False)
                        return e.matmul(PS[psn][:, 0:n], SEL[0:72, h, :], FR[0:72, qs], start=False, stop=True)
                    P.op("pe", fn, [("fqh", hb), ("fkh", hb), "fr"], [("ps", psn)])
                    r = pi % 3
                    pi += 1
                    act(PTL[r][:, 0:n], PS[psn][:, 0:n], AF.Exp, [("ps", psn), "gtt"], [("ptl", r)],
                        bias=GTT[:, kb * 8 + h:kb * 8 + h + 1], scale=1.0)

                    def fn2(e, po=po, pd=pd, kb=kb, c0=c0, n=n, r=r, nkb=nkb, FVH=FVH):
                        e.matmul(PS[po][:, c0:SUB], FVH[:, kb, :], PTL[r][:, 0:n], start=(kb == 0), stop=(kb == nkb - 1))
                        return e.matmul(PS[pd][:, c0:SUB], ONESB, PTL[r][:, 0:n], start=(kb == 0), stop=(kb == nkb - 1))
                    P.op("pe", fn2, [("ptl", r), ("fvh", hb)], [("ps", po), ("ps", pd)])
                recip(RDEN[:], PS[pd][:], [("ps", pd)], ["rden"])
                r = yi % 2
                yi += 1
                tt(YST[r][:], PS[po][:], RDEN[:], ALU.mult, [("ps", po), "rden"], [("yst", r)])
                dma(YFS[:, h, qg * SUB:(qg + 1) * SUB], YST[r][:], [("yst", r)], (), f"yst{r}")
        P.barrier()

    def m3r_stage(l):
        off = cur[0]
        limit[0] = top
        RQH = alloc("rqh", [128, NB, 128], BF16)
        RKH = alloc("rkh", [128, NB, 128], BF16)
        RVH = alloc("rvh", [128, NB, 128], BF16)
        RGH = alloc("rgh", [128, S], BF16)
        RQT = alloc("rqt", [128, S], BF16)
        RQXT = alloc("rqxt", [128, S], BF16)
        RKT = alloc("rkt", [128, S], BF16)
        SDEC = alloc("sdec", [128, NB, 128], BF16)
        STB = alloc("stb", [128, NB, 128], BF16)
        STATE = alloc("state", [128, 128], F32)
        OF = alloc("of", [128, SUB], F32)
        OB = alloc("ob", [128, SUB], BF16)
        OSQ = alloc("osq", [128, SUB], BF16)
        MN = alloc("mn", [128, SUB], F32)
        T1 = alloc("t1", [128, SUB], F32)
        VAR = alloc("var", [128, SUB], F32)
        CC = alloc("cc", [128, SUB], F32)
        YST = [alloc(f"yst{i}", [128, SUB], BF16) for i in range(2)]
        cur[0] = off
        yi = 0
        RLV = int(os.environ.get("MK_R", "9"))
        XV = int(os.environ.get("MK_X", "0"))
        for h in range(H):
            cd = float(np.exp(128.0 * log_gamma(h)))
            hs = slice(h * 128, (h + 1) * 128)
            dma(RQH[:], RQS[:, :, hs], (), ["rqh", "hchain"], "hld0")
            dma(RKH[:], RKS[:, :, hs], (), ["rkh", "hchain"], "hld0")
            dma(RVH[:], RVS[:, :, hs], (), ["rvh", "hchain"], "hld0")
            dma(RGH[:], RGS[:, h, :], (), ["rgh", "hchain"], "hld0")
            if RLV < 1:
                continue
            for grp in range(4):
                gsl = slice(grp * SUB, (grp + 1) * SUB)
                a = next_pt()

                def fnq(e, a=a, grp=grp):
                    ins = None
                    for j in range(4):
                        ins = e.matmul(PS[a][:, j * 128:(j + 1) * 128], RQH[:, grp * 4 + j, :], IDENT, start=True, stop=True)
                    return ins
                P.op("pe", fnq, ["rqh"], [("ps", a)])
                if XV != 2:
                    cp(RQT[:, gsl], PS[a][:], [("ps", a)], [("rqt", grp)])
                for j in range(4 if XV != 1 else 0):
                    n = grp * 4 + j
                    tt(RQXT[:, n * 128:(n + 1) * 128], PS[a][:, j * 128:(j + 1) * 128], XIROW[:, h, :], ALU.mult,
                       [("ps", a)], [("rqxt", n)])
                if XV != 0:
                    continue
                a2 = next_pt()

                def fnk(e, a2=a2, grp=grp):
                    ins = None
                    for j in range(4):
                        ins = e.matmul(PS[a2][:, j * 128:(j + 1) * 128], RKH[:, grp * 4 + j, :], IDENT, start=True, stop=True)
                    return ins
                P.op("pe", fnk, ["rkh"], [("ps", a2)])
                act(RKT[:, gsl], PS[a2][:], AF.Copy, [("ps", a2)], [("rkt", grp)])
            if RLV < 2:
                continue
            for grp in range(4):
                p = grp % 2

                def fns(e, p=p, grp=grp):
                    ins = None
                    for j in range(4):
                        ns = slice((grp * 4 + j) * 128, (grp * 4 + j + 1) * 128)
                        ins = e.matmul(PS[p][:, j * 128:(j + 1) * 128], RKT[:, ns], RQT[:, ns], start=True, stop=True)
                    return ins
                P.op("pe", fns, [("rkt", grp), ("rqt", grp)], [("ps", p)])
                for j in range(4):
                    n = grp * 4 + j
                    tt(SDEC[:, n, :], PS[p][:, j * 128:(j + 1) * 128], DTAB[:, h, :], ALU.mult, [("ps", p)], [("sdec", n)])
            if RLV < 3:
                continue
            memset(STATE[:], 0.0, ["state"])
            kvp = [2, 3, 4, 5]
            for grp in range(4):
                p = kvp[grp]

                def fnkv(e, p=p, grp=grp):
                    ins = None
                    for j in range(4):
                        n = grp * 4 + j
                        ins = e.matmul(PS[p][:, j * 128:(j + 1) * 128], RKH[:, n, :], RVH[:, n, :], start=True, stop=True)
                    return ins
                P.op("pe", fnkv, ["rkh", "rvh"], [("ps", p)])
            for n in range(NB):
                cp(STB[:, n, :], STATE[:], ["state"], [("stb", n)])
                p, j = kvp[n // 4], n % 4
                stt(STATE[:], STATE[:], cd, PS[p][:, j * 128:(j + 1) * 128], ALU.mult, ALU.add,
                    ["state", ("ps", p)], ["state"])
            if RLV < 4:
                continue
            for grp in range(4):
                gsl = slice(grp * SUB, (grp + 1) * SUB)
                po = grp % 2

                def fno(e, po=po, grp=grp):
                    ins = None
                    for j in range(4):
                        n = grp * 4 + j
                        ns = slice(n * 128, (n + 1) * 128)
                        e.matmul(PS[po][:, j * 128:(j + 1) * 128], RVH[:, n, :], SDEC[:, n, :], start=True, stop=False)
                        ins = e.matmul(PS[po][:, j * 128:(j + 1) * 128], STB[:, n, :], RQXT[:, ns], start=False, stop=True)
                    return ins
                P.op("pe", fno, ["rvh"] + [(k, grp * 4 + j) for j in range(4) for k in ("sdec", "stb", "rqxt")], [("ps", po)])
                pok = [("ps", po)]
                act(OF[:], PS[po][:], AF.Copy, pok, ["of"])
                act(OB[:], PS[po][:], AF.Copy, pok, ["ob"])
                act(OSQ[:], PS[po][:], AF.Square, pok, ["osq"])
                if RLV < 5:
                    continue
                pm, pq = 1 - (grp % 2), kvp[grp]
                mm(PS[pm][:], [(ONES128, OB[:])], ["ob"], [("ps", pm)])
                mm(PS[pq][:], [(ONES128, OSQ[:])], ["osq"], [("ps", pq)])
                act(MN[:], PS[pm][:], AF.Copy, [("ps", pm)], ["mn"])
                tt(T1[:], MN[:], MN[:], ALU.mult, ["mn"], ["t1"])
                stt(VAR[:], T1[:], -1.0, PS[pq][:], ALU.mult, ALU.add, ["t1", ("ps", pq)], ["var"])
                ts(VAR[:], VAR[:], 0.0, ALU.max, ["var"], ["var"])
                act(T1[:], VAR[:], AF.Sqrt, ["var", "t1"], ["t1"], bias=EPS_GN[:, 0:1], scale=1.0)
                recip(VAR[:], T1[:], ["t1", "var"], ["var"])
                tt(CC[:], OF[:], MN[:], ALU.subtract, ["of", "mn"], ["cc"])
                tt(CC[:], CC[:], VAR[:], ALU.mult, ["cc", "var"], ["cc"])
                r = yi % 2
                yi += 1
                tt(YST[r][:], CC[:], RGH[:, gsl], ALU.mult, ["cc", "rgh"], [("yst", r)])
                dma(YRS[:, h, gsl], YST[r][:], [("yst", r)], (), f"yst{r}")
        P.barrier()

    def m4_stage(l):
        off = cur[0]
        limit[0] = top
        MG = alloc("mg", [128, KC, TT], BF16)
        SG = [alloc(f"sg{i}", [128, SUB], F32) for i in range(3)]
        MACC = [alloc(f"macc{i}", [128, SUB], F32) for i in range(2)]
        TMP = [alloc(f"tmp{i}", [128, SUB], F32) for i in range(2)]
        cur[0] = off
        YT = [alloc(f"yt{i}", [128, 8, TT], BF16, at=offs["xt0"] + i * 8 * TT * 2) for i in range(3)]
        ysrc = [YPS, YFS, YRS]
        for t in range(NT):
            tsl = slice(t * TT, (t + 1) * TT)
            dma(XN[:], XNS[:, :, tsl], (), xnk_all, "xnld")
            for i in range(3):
                dma(YT[i][:], ysrc[i][:, :, tsl], (), [("yt", i)], f"yld{i}")
            for dc in range(KC):
                r0 = (l * 16 + dc) * 128
                wa, wak = W.get(wgad[r0:r0 + 128, :], 4096)
                wb, wbk = W.get(wgbd[r0:r0 + 128, :], 2048, group=True)
                wr, wrk = W.get(wbrd[r0:r0 + 128, :], 3072, group=True)
                wa3 = wa[:, 0:4096].rearrange("p (k c) -> p k c", k=KC)
                wb3 = wb[:, 0:2048].rearrange("p (k c) -> p k c", k=KC)
                wr4 = wr[:, 0:3072].rearrange("p (b c j) -> p b c j", b=3, c=8)
                for br in range(3):
                    for sub in range(NS):
                        pg, pb = next_ps(), next_ps()
                        if br < 2:
                            mm(PS[pg][:], [(wa3[:, kc, br * 128:(br + 1) * 128], XN[:, kc, ssl(sub)]) for kc in range(KC)],
                               [wak] + xnk(sub), [("ps", pg)])
                        else:
                            mm(PS[pg][:], [(wb3[:, kc, :], XN[:, kc, ssl(sub)]) for kc in range(KC)], [wbk] + xnk(sub), [("ps", pg)])
                        mm(PS[pb][:], [(wr4[:, br, c, :], YT[br][:, c, ssl(sub)]) for c in range(8)], [wrk, ("yt", br)], [("ps", pb)])
                        act(SG[br][:], PS[pg][:], AF.Sigmoid, [("ps", pg)], [("sg", br)])
                        if br == 0:
                            tt(MACC[sub][:], SG[0][:], PS[pb][:], ALU.mult, [("sg", 0), ("ps", pb)], [("macc", sub)])
                        elif br == 1:
                            tt(TMP[0][:], SG[1][:], PS[pb][:], ALU.mult, [("sg", 1), ("ps", pb)], [("tmp", 0)])
                            tt(MACC[sub][:], MACC[sub][:], TMP[0][:], ALU.add, [("macc", sub), ("tmp", 0)], [("macc", sub)])
                        else:
                            tt(TMP[1][:], SG[2][:], PS[pb][:], ALU.mult, [("sg", 2), ("ps", pb)], [("tmp", 1)])
                            tt(MG[:, dc, ssl(sub)], MACC[sub][:], TMP[1][:], ALU.add, [("macc", sub), ("tmp", 1)], [("mg", dc, sub)])
            P.barrier()
            dma(XT[0][:], XS[:, :, tsl], (), xtk, "xld0")
            for dp in range(8):
                r0 = (l * 8 + dp) * 128
                w, wk = W.get(woutd[r0:r0 + 128, :], 4096)
                w3 = w[:, 0:4096].rearrange("p (k c) -> p k c", k=KC)
                for j in range(2):
                    dc = dp * 2 + j
                    for sub in range(NS):
                        p = next_ps()
                        mm(PS[p][:], [(w3[:, kc, j * 128:(j + 1) * 128], MG[:, kc, ssl(sub)]) for kc in range(KC)],
                           [wk] + [("mg", kc, sub) for kc in range(KC)], [("ps", p)])
                        tt(XT[0][:, dc, ssl(sub)], PS[p][:], XT[0][:, dc, ssl(sub)], ALU.add,
                           [("ps", p), ("xt", 0, dc, sub)], [("xt", 0, dc, sub)])
            dma(XS[:, :, tsl], XT[0][:], xtk, (), "xst0")
            P.barrier()

    GS = alloc("gs", [128, S], F32, at=GS_OFF)
    FR = alloc("fr", [128, S], BF16, at=GS_OFF + 8192)
    GTT = alloc("gtt", [128, 128], F32)
    ONE_C = alloc("onec", [128, 2], F32)
    EPS_GN = alloc("epsg", [128, 2], F32)
    ov_base = cur[0]

    def trace():
        W.cons = 0
        W.safe = 0
        psi[0] = 0
        pti[0] = 0
        dma(CF[:], cfd[:, :], (), ["cf"], "cf")
        dma(CB[:], cbd[:, :], (), ["cb"], "cb")
        ts(NFB[:], FBR, -1.0, ALU.mult, ["cf"], ["nfb"])
        memset(EPS_RMS[:], RMS_EPS, ["epsr"])
        memset(EPS_GN[:], GN_EPS, ["epsg"])
        memset(ONE_C[:], 1.0, ["onec"])
        P.barrier()
        st = os.environ.get("MK_STAGES", "f0,m1,m2,m3,m3r,m4,f1").split(",")
        for l in range(L):
            if "f0" in st:
                ffn_stage(l, 0, xin if l == 0 else XS, XS, False)
            if "m1" in st:
                m1_stage(l)
            if "m2" in st:
                m2_stage(l)
            if "m3" in st:
                m3_stage(l)
            if "m3r" in st:
                m3r_stage(l)
            if "m4" in st:
                m4_stage(l)
            if "f1" in st:
                ffn_stage(l, 1, XS, xout if l == L - 1 else XS, l == L - 1)

    P.dry = True
    trace()
    P.dry = False
    trace()
    P.barrier()
    P.emit()
    return nc


def _consts(L):
    NG = L * 3 + 1
    lg = np.array([np.log1p(-np.power(2.0, -5.0 - h)) for h in range(H)], np.float64)
    p = np.arange(128, dtype=np.float64)
    zeta = (128.0 ** -0.5) * np.exp((127.0 - p)[:, None] * lg[None, :])
    i8 = np.zeros((128, 8)); i8[:8, :8] = np.eye(8)
    q = np.arange(128, dtype=np.float64)
    dtab = np.zeros((128, 8, 128))
    for h in range(H):
        m = (p[:, None] <= q[None, :])
        dtab[:, h, :] = np.where(m, np.exp((q[None, :] - 127.0) * lg[h]), 0.0)
    xirow = np.zeros((128, 8, 128))
    for h in range(H):
        xirow[:, h, :] = np.exp((q + 1.0) * lg[h])[None, :]
    invc = np.zeros((128, 4, 16))
    for g in range(4):
        w = 2 ** (g + 1)
        invc[:, g, :] = 1.0 / np.minimum(np.arange(16) + 1, w)[None, :]
    tail = np.concatenate([zeta, i8, dtab.reshape(128, -1), xirow.reshape(128, -1), invc.reshape(128, -1)], axis=1)
    cb = np.zeros((128, 4 * 128 + 8 * 128), np.float32)
    cb[:, 0:128] = 1.0
    cb[:, 128:256] = 1.0 / 128.0
    cb[:, 256:384] = np.eye(128)
    cb[:, 384:512] = np.where(p[:, None] > q[None, :], -30000.0, 0.0)
    sel = np.zeros((128, 8, 128), np.float32)
    for h in range(H):
        for r in (h, 32 + h, 64 + h):
            sel[r, h, :] = -1.0
    cb[:, 512:] = sel.reshape(128, -1)
    half = 64
    inv_freq = (np.float32(10000.0) ** (-np.arange(half, dtype=np.float32) / np.float32(half))).astype(np.float32)
    pos = np.arange(S, dtype=np.float32)
    ang = (pos[:, None] * inv_freq[None, :]).astype(np.float32).astype(np.float64)
    cos, sin = np.cos(ang), np.sin(ang)
    cos2 = np.concatenate([cos, cos], axis=1)
    sins = np.concatenate([-sin, sin], axis=1)
    cosr = np.tile(cos2.reshape(NB, 128, 1, 128), (1, 1, 2, 1)).transpose(1, 0, 2, 3).reshape(128, NB, 256)
    sinr = np.tile(sins.reshape(NB, 128, 1, 128), (1, 1, 2, 1)).transpose(1, 0, 2, 3).reshape(128, NB, 256)
    return tail.astype(np.float32), cb.astype(ml_dtypes.bfloat16), np.ascontiguousarray(cosr, np.float32), np.ascontiguousarray(sinr, np.float32)


def _fm(v):
    return v.reshape(v.shape[:-1] + (KC, 128))


def _layout_weights(inp, ls):
    L = len(ls)
    w13 = np.empty((L, 2, FC, 128, KC, 2, 128), np.float32)
    w2 = np.empty((L, 2, 16, 2, 128, 22, 128), np.float32)
    for i, l in enumerate(ls):
        for wi, (a, b) in enumerate((("ffn1_w13", "ffn1_w2"), ("ffn2_w13", "ffn2_w2"))):
            w13[i, wi] = inp[a][l].reshape(KC, 128, 2, FC, 128).transpose(3, 1, 0, 2, 4)
            w2[i, wi] = inp[b][l].reshape(2, 22, 128, 16, 128).transpose(3, 0, 2, 1, 4)
    win = np.empty((L, 32, 128, KC, 256), np.float32)
    wfl = np.zeros((L, 128, KC, 72), np.float32)
    wga = np.empty((L, 16, 128, KC, 2, 128), np.float32)
    wgb = np.empty((L, 16, 128, KC, 128), np.float32)
    wbr = np.empty((L, 16, 128, 3, 8, 128), np.float32)
    wout = np.empty((L, 8, 128, KC, 256), np.float32)
    pw = np.empty((L, 4, 128, 2, 256), np.float32)
    order = [OFF_POOL, OFF_FQ, OFF_FK, OFF_RG, OFF_FV, OFF_RV, OFF_RQ, OFF_RK]
    for i, l in enumerate(ls):
        wi_ = inp["w_in"][l]
        for fi, off in enumerate(order):
            win[i, fi * 4:(fi + 1) * 4] = wi_[:, off:off + 1024].reshape(KC, 128, 4, 256).transpose(2, 1, 0, 3)
        fl = wi_[:, OFF_FL:OFF_FL + 8].reshape(KC, 128, 8).transpose(1, 0, 2)
        for r in (0, 32, 64):
            wfl[i, :, :, r:r + 8] = fl
        gts = wi_[:, OFF_GATE:OFF_GATE + 3 * D]
        wga[i] = gts[:, 0:2 * D].reshape(KC, 128, 2, 16, 128).transpose(3, 1, 0, 2, 4)
        wgb[i] = gts[:, 2 * D:3 * D].reshape(KC, 128, 16, 128).transpose(2, 1, 0, 3)
        br = np.stack([inp["w_branch_pool"][l], inp["w_branch_fox"][l], inp["w_branch_ret"][l]], 0)
        wbr[i] = br.reshape(3, 8, 128, 16, 128).transpose(3, 2, 0, 1, 4)
        wout[i] = inp["w_out"][l].reshape(KC, 128, 8, 256).transpose(2, 1, 0, 3)
        pw[i] = inp["pool_w"][l].reshape(4, 2, 128, 256).transpose(0, 2, 1, 3)
    NG = L * 3 + 1
    gains = np.zeros((NG, D), np.float32)
    for i, l in enumerate(ls):
        gains[i * 3 + 0] = inp["ffn1_norm"][l]
        gains[i * 3 + 1] = inp["mix_norm"][l]
        gains[i * 3 + 2] = inp["ffn2_norm"][l]
    gains[L * 3] = inp["final_norm"]
    gain_fm = gains.reshape(NG, KC, 128).transpose(2, 0, 1).reshape(128, NG * KC)
    psc = np.stack([inp["pool_scale"][l] for l in ls], 0).reshape(L, 8, 128).transpose(2, 0, 1).reshape(128, L * 8)
    fbr = np.zeros((128, L), np.float32)
    for i, l in enumerate(ls):
        for r in (0, 32, 64):
            fbr[r:r + 8, i] = inp["forget_bias"][l]
    tail, cb, cosr, sinr = _consts(L)
    cf = np.ascontiguousarray(np.concatenate([gain_fm, psc, fbr, tail], axis=1), np.float32)
    return {
        "w13": w13.reshape(-1, 4096), "w2": w2.reshape(-1, 2816), "win": win.reshape(-1, 4096),
        "wfl": wfl.reshape(-1, KC * 72), "wga": wga.reshape(-1, 4096), "wgb": wgb.reshape(-1, 2048),
        "wbr": wbr.reshape(-1, 3072), "wout": wout.reshape(-1, 4096), "pw": pw.reshape(-1, 512),
        "cf": cf, "cb": cb, "cosr": cosr, "sinr": sinr,
    }


_NC_CACHE = {}


def _get_nc(nl):
    if nl not in _NC_CACHE:
        _NC_CACHE[nl] = build(nl)
    return _NC_CACHE[nl]


LAYERS_PER_LAUNCH = int(os.environ.get("MK_LPL", "4"))
N_LAYERS = int(os.environ.get("MK_NL", str(DEPTH)))


def kernel(**inp):
    inp = {k: np.asarray(v) for k, v in inp.items()}
    B = inp["x"].shape[0]
    xs = [np.ascontiguousarray(inp["x"][b].T.reshape(KC, 128, S).transpose(1, 0, 2)) for b in range(B)]
    ys = None
    l0 = 0
    while l0 < N_LAYERS:
        ls = list(range(l0, min(N_LAYERS, l0 + LAYERS_PER_LAUNCH)))
        nc = _get_nc(len(ls))
        wl = _layout_weights(inp, ls)
        in_maps = [dict(wl, xin=xs[b]) for b in range(B)]
        res = run_bass_kernel_spmd(nc, in_maps, core_ids=list(range(B)))
        xs = [np.ascontiguousarray(res.results[b]["xout"]) for b in range(B)]
        if os.environ.get("MK_DEBUG") == "1":
            global LAST_RES
            LAST_RES = res.results
        ys = [res.results[b]["yout"] for b in range(B)]
        l0 += len(ls)
    out = np.stack([y.transpose(1, 0, 2).reshape(D, S).T for y in ys], 0)
    return np.ascontiguousarray(out, np.float32)
```

```python
import os
import numpy as np
import ml_dtypes
import concourse.bass as bass
import concourse.mybir as mybir
from concourse.bass_utils import run_bass_kernel_spmd

F32 = mybir.dt.float32
BF16 = mybir.dt.bfloat16
ALU = mybir.AluOpType
AF = mybir.ActivationFunctionType

D = 2048
S = 2048
KC = 16
TT = 1024
NT = S // TT
SUB = 512
NS = TT // SUB
NQ = S // SUB
DFF = 5632
FC = 44
H = 8
NB = S // 128
DEPTH = 4
RMS_EPS = 1e-6
GN_EPS = 1e-5
OFF_POOL, OFF_FQ, OFF_FK, OFF_FV, OFF_FL, OFF_RQ, OFF_RK, OFF_RV, OFF_RG, OFF_GATE = (
    0, 1024, 2048, 3072, 4096, 4104, 5128, 6152, 7176, 8200)
NSLOT = 5
STRICT = int(os.environ.get("MK_STRICT", "1"))
SLOT_ELEMS = 4096


class Op:
    __slots__ = ("eng", "fn", "deps", "dsem", "signal", "sem", "inc", "waits", "val")

    def __init__(self, eng, fn, deps, dsem):
        self.eng, self.fn, self.deps, self.dsem = eng, fn, deps, dsem
        self.signal = dsem is not None
        self.sem = None
        self.inc = 0
        self.val = 0
        self.waits = []


class Prog:
    ENGS = ("pe", "act", "dve", "pool", "sp")

    def __init__(self, nc):
        self.nc = nc
        self.ops = []
        self.lw = {}
        self.rd = {}
        self.dry = False
        self.last_eng = {}
        self.last_dma = {}

    def op(self, eng, fn, reads=(), writes=(), dsem=None):
        if self.dry:
            return
        i = len(self.ops)
        deps = set()
        for k in reads:
            j = self.lw.get(k)
            if j is not None:
                deps.add(j)
        for k in writes:
            j = self.lw.get(k)
            if j is not None:
                deps.add(j)
            deps.update(self.rd.get(k, ()))
        for k in reads:
            self.rd.setdefault(k, []).append(i)
        for k in writes:
            self.lw[k] = i
            self.rd[k] = []
        self.ops.append(Op(eng, fn, deps, dsem))
        if dsem is None:
            self.last_eng[eng] = i
        else:
            self.last_dma[dsem] = i

    def barrier(self):
        if self.dry:
            return
        deps = set(self.last_eng.values()) | set(self.last_dma.values())
        for e in self.ENGS:
            self.ops.append(Op(e, None, set(deps), None))
        self.lw = {}
        self.rd = {}

    def finalize(self):
        nc = self.nc
        ops = self.ops
        for o in ops:
            for j in o.deps:
                d = ops[j]
                if d.dsem is None and (STRICT or d.eng != o.eng):
                    d.signal = True
        esem = {e: nc.alloc_semaphore("s_" + e) for e in self.ENGS}
        dsems = {}
        ecnt = {e: 0 for e in self.ENGS}
        dcnt = {}
        for o in ops:
            if o.dsem is not None:
                if o.dsem not in dsems:
                    dsems[o.dsem] = nc.alloc_semaphore("d_" + o.dsem)
                    dcnt[o.dsem] = 0
                dcnt[o.dsem] += 16
                o.sem, o.inc, o.val = dsems[o.dsem], 16, dcnt[o.dsem]
            elif o.signal:
                ecnt[o.eng] += 1
                o.sem, o.inc, o.val = esem[o.eng], 1, ecnt[o.eng]
        waited = {e: {} for e in self.ENGS}
        for o in ops:
            need = {}
            for j in o.deps:
                d = ops[j]
                if d.dsem is None and d.eng == o.eng and (not STRICT or o.eng == "pe"):
                    continue
                key = id(d.sem)
                if key not in need or need[key][1] < d.val:
                    need[key] = (d.sem, d.val)
            w = waited[o.eng]
            for key, (sem, val) in need.items():
                if w.get(key, 0) >= val:
                    continue
                w[key] = val
                o.waits.append((sem, val))
        self.by_eng = {e: [o for o in ops if o.eng == e] for e in self.ENGS}
        if os.environ.get("MK_VERBOSE"):
            print("sem counts", ecnt, "dma", {k: v for k, v in dcnt.items()}, "nops", len(ops), flush=True)

    def emit(self):
        nc = self.nc
        self.finalize()

        def mk(name):
            def body(e):
                for o in self.by_eng[name]:
                    for sem, val in o.waits:
                        e.wait_ge(sem, val)
                    if o.fn is not None:
                        ins = o.fn(e)
                        if o.signal:
                            ins.then_inc(o.sem, o.inc)
            return body

        with nc.Block() as block:
            block.tensor(mk("pe"))
            block.scalar(mk("act"))
            block.vector(mk("dve"))
            block.gpsimd(mk("pool"))
            block.sync(mk("sp"))


def log_gamma(h):
    return float(np.log1p(-np.power(2.0, -5.0 - h)))


def build(nlayers):
    nc = bass.Bass("TRN2", target_bir_lowering=False)
    P = Prog(nc)
    L = nlayers

    def din(name, shape, dt=F32):
        return nc.dram_tensor(name, shape, dt, kind="ExternalInput").ap()

    xin = din("xin", [128, KC, S])
    w13d = din("w13", [L * 2 * FC * 128, 4096])
    w2d = din("w2", [L * 2 * 32 * 128, 2816])
    wind = din("win", [L * 32 * 128, 4096])
    wfld = din("wfl", [L * 128, KC * 72])
    wgad = din("wga", [L * 16 * 128, 4096])
    wgbd = din("wgb", [L * 16 * 128, 2048])
    wbrd = din("wbr", [L * 16 * 128, 3072])
    woutd = din("wout", [L * 8 * 128, 4096])
    pwd = din("pw", [L * 4 * 128, 512])
    NG = L * 3 + 1
    NCF = NG * 16 + L * 8 + L + 8 + 8 + 8 * 128 + 8 * 128 + 64
    cfd = din("cf", [128, NCF])
    NCB = 4 * 128 + 8 * 128
    cbd = din("cb", [128, NCB], BF16)
    cosd = din("cosr", [128, NB, 256])
    sind = din("sinr", [128, NB, 256])
    xout = nc.dram_tensor("xout", [128, KC, S], F32, kind="ExternalOutput").ap()
    yout = nc.dram_tensor("yout", [128, KC, S], F32, kind="ExternalOutput").ap()

    DBG = os.environ.get("MK_DEBUG") == "1"

    def dscr(name, shape, dt=BF16):
        if DBG and name != "XS":
            return nc.dram_tensor(name, shape, dt, kind="ExternalOutput").ap()
        return nc.dram_tensor(name, shape, dt).ap()

    XS = dscr("XS", [128, KC, S], F32)
    XNS = dscr("XNS", [128, KC, S])
    US = dscr("US", [128, 8, S])
    FQS = dscr("FQS", [128, 8, S])
    FKS = dscr("FKS", [128, 8, S])
    RGS = dscr("RGS", [128, 8, S])
    FVS = dscr("FVS", [128, NB, 1024])
    RVS = dscr("RVS", [128, NB, 1024])
    RQS = dscr("RQS", [128, NB, 1024])
    RKS = dscr("RKS", [128, NB, 1024])
    YPS = dscr("YPS", [128, 8, S])
    YFS = dscr("YFS", [128, 8, S])
    YRS = dscr("YRS", [128, 8, S])

    base = (nc.sbuf_base + 63) // 64 * 64
    top = nc.sbuf_top
    cur = [base]

    uid = [0]
    offs = {}
    limit = [top]
    GS_OFF = (top - 8192 - 4096 - 128) // 64 * 64

    def alloc(name, shape, dt, at=None):
        uid[0] += 1
        name = f"{name}_{uid[0]}"
        nbytes = int(np.prod(shape[1:])) * (4 if dt == F32 else 2)
        if at is None:
            off = cur[0]
            cur[0] = (off + nbytes + 63) // 64 * 64
            assert off + nbytes <= limit[0], (name, off, nbytes, limit[0])
        else:
            off = at
        assert off + nbytes <= top, (name, off, nbytes, top)
        offs[name.rsplit("_", 1)[0]] = off
        return nc.alloc_sbuf_tensor_at(name, list(shape), dt, offset=off)

    WSL = [alloc(f"ws{i}", [128, SLOT_ELEMS], BF16) for i in range(NSLOT)]
    XT = [alloc("xt0", [128, KC, TT], F32)]
    XN = alloc("xn", [128, KC, TT], BF16)
    CF = alloc("cf", [128, NCF], F32)
    CB = alloc("cb", [128, NCB], BF16)
    NFB = alloc("nfb", [128, L], F32)
    SQ = [alloc(f"sq{i}", [128, SUB], BF16) for i in range(2)]
    RMS = alloc("rms", [128, SUB], F32)
    RSTD = alloc("rstd", [128, SUB], F32)
    ov_base = cur[0]

    o = 0
    GAIN = CF[:, o:o + NG * 16].rearrange("p (g k) -> p g k", k=16); o += NG * 16
    PSC = CF[:, o:o + L * 8].rearrange("p (l c) -> p l c", c=8); o += L * 8
    FBR = CF[:, o:o + L]; o += L
    ZETA = CF[:, o:o + 8]; o += 8
    I8 = CF[:, o:o + 8]; o += 8
    DTAB = CF[:, o:o + 1024].rearrange("p (h q) -> p h q", h=8); o += 1024
    XIROW = CF[:, o:o + 1024].rearrange("p (h q) -> p h q", h=8); o += 1024
    INVC = CF[:, o:o + 64].rearrange("p (g j) -> p g j", g=4); o += 64
    ONESB = CB[:, 0:128]
    ONES128 = CB[:, 128:256]
    IDENT = CB[:, 256:384]
    NEGM = CB[:, 384:512]
    SEL = CB[:, 512:512 + 1024].rearrange("p (h m) -> p h m", h=8)

    PS = [nc.alloc_psum_tensor(f"ps{i}", [128, 512], F32) for i in range(6)]
    psi = [0]
    pti = [0]

    def next_ps():
        i = psi[0] % 6
        psi[0] += 1
        return i

    def next_pt():
        i = 2 + pti[0] % 4
        pti[0] += 1
        return i

    class WStream:
        def __init__(self):
            self.reqs = []
            self.cons = 0
            self.issued = 0
            self.safe = 0

        def get(self, src, n, group=False):
            j = self.cons
            self.cons += 1
            if not group:
                self.safe = j
            if P.dry:
                self.reqs.append((src, n))
            else:
                while self.issued < min(len(self.reqs), j + NSLOT) and self.issued - NSLOT < self.safe:
                    i = self.issued
                    s = i % NSLOT
                    src_i, n_i = self.reqs[i]
                    P.op("pool", lambda e, s=s, src_i=src_i, n_i=n_i: e.dma_start(out=WSL[s][:, 0:n_i], in_=src_i),
                         reads=(), writes=[("ws", s)], dsem=f"ws{s}")
                    self.issued += 1
            return WSL[j % NSLOT], ("ws", j % NSLOT)

    W = WStream()

    def mm(out, pairs, reads, writes, start=True, stop=True):
        def fn(e, out=out, pairs=pairs, start=start, stop=stop):
            n = len(pairs)
            ins = None
            for i, (l, r) in enumerate(pairs):
                ins = e.matmul(out, l, r, start=(start and i == 0), stop=(stop and i == n - 1))
            return ins
        P.op("pe", fn, reads, writes)

    def act(out, in_, func, reads, writes, bias=None, scale=None):
        def fn(e):
            kw = {}
            if bias is not None:
                kw["bias"] = bias
            if scale is not None:
                kw["scale"] = scale
            return e.activation(out=out, in_=in_, func=func, **kw)
        P.op("act", fn, reads, writes)

    def tt(out, a, b, op, reads, writes, eng="dve"):
        P.op(eng, lambda e: e.tensor_tensor(out=out, in0=a, in1=b, op=op), reads, writes)

    def stt(out, a, sc, b, op0, op1, reads, writes, eng="dve"):
        P.op(eng, lambda e: e.scalar_tensor_tensor(out=out, in0=a, scalar=sc, in1=b, op0=op0, op1=op1), reads, writes)

    def ts(out, a, s1, op0, reads, writes, s2=None, op1=None, eng="dve"):
        def fn(e):
            if op1 is None:
                return e.tensor_scalar(out=out, in0=a, scalar1=s1, scalar2=None, op0=op0)
            return e.tensor_scalar(out=out, in0=a, scalar1=s1, scalar2=s2, op0=op0, op1=op1)
        P.op(eng, fn, reads, writes)

    def cp(out, in_, reads, writes, eng="dve"):
        P.op(eng, lambda e: e.tensor_copy(out=out, in_=in_), reads, writes)

    def recip(out, in_, reads, writes):
        P.op("dve", lambda e: e.reciprocal(out=out, in_=in_), reads, writes)

    def memset(ap, v, writes, eng="dve"):
        P.op(eng, lambda e: e.memset(ap, v), (), writes)

    def dma(out, in_, reads, writes, dsem, eng="sp"):
        P.op(eng, lambda e: e.dma_start(out=out, in_=in_), reads, writes, dsem=dsem)

    xtk = [("xt", 0, k, sb) for k in range(KC) for sb in range(NS)]
    xnk_all = [("xn", k, sb) for k in range(KC) for sb in range(NS)]

    def xnk(sub):
        return [("xn", k, sub) for k in range(KC)]

    def ssl(sub):
        return slice(sub * SUB, (sub + 1) * SUB)

    def norm_sub(gi, sub, out_fn, out_keys_fn):
        pss = next_ps()
        for kc in range(KC):
            act(SQ[kc % 2][:], XT[0][:, kc, ssl(sub)], AF.Square, [("xt", 0, kc, sub)], [("sq", kc % 2)])
            mm(PS[pss][:], [(ONESB, SQ[kc % 2][:])], [("sq", kc % 2)], [("ps", pss)], start=(kc == 0), stop=(kc == KC - 1))
        act(RMS[:], PS[pss][:], AF.Sqrt, [("ps", pss)], ["rms"], bias=EPS_RMS[:, 0:1], scale=1.0 / D)
        recip(RSTD[:], RMS[:], ["rms"], ["rstd"])
        for kc in range(KC):
            stt(out_fn(kc), XT[0][:, kc, ssl(sub)], GAIN[:, gi, kc:kc + 1], RSTD[:], ALU.mult, ALU.mult,
                [("xt", 0, kc, sub), "rstd"], out_keys_fn(kc))

    def norm_xn(gi):
        for sub in range(NS):
            norm_sub(gi, sub, lambda kc, sub=sub: XN[:, kc, ssl(sub)], lambda kc, sub=sub: [("xn", kc, sub)])

    EPS_RMS = alloc("epsr", [128, 2], F32)

    def ffn_stage(l, which, src, dst, final):
        off = cur[0]
        limit[0] = top
        GT = alloc("gT", [128, 22, TT], BF16)
        SA = [alloc(f"sa{i}", [128, SUB], F32) for i in range(2)]
        cur[0] = off
        gi = l * 3 + (0 if which == 0 else 2)
        si = 0
        for t in range(NT):
            tsl = slice(t * TT, (t + 1) * TT)
            dma(XT[0][:], src[:, :, tsl], (), xtk, "xld0")
            norm_xn(gi)
            for hh in range(2):
                for f in range(22):
                    fg = hh * 22 + f
                    r0 = ((l * 2 + which) * FC + fg) * 128
                    w, wk = W.get(w13d[r0:r0 + 128, :], 4096)
                    w3 = w[:, 0:4096].rearrange("p (k c) -> p k c", k=KC)
                    for sub in range(NS):
                        pa, pb = next_ps(), next_ps()
                        mm(PS[pa][:], [(w3[:, kc, 0:128], XN[:, kc, ssl(sub)]) for kc in range(KC)], [wk] + xnk(sub), [("ps", pa)])
                        mm(PS[pb][:], [(w3[:, kc, 128:256], XN[:, kc, ssl(sub)]) for kc in range(KC)], [wk] + xnk(sub), [("ps", pb)])
                        r = si % 2
                        si += 1
                        act(SA[r][:], PS[pa][:], AF.Silu, [("ps", pa)], [("sa", r)])
                        tt(GT[:, f, ssl(sub)], SA[r][:], PS[pb][:], ALU.mult, [("sa", r), ("ps", pb)], [("gT", f, sub)])
                for dc in range(KC):
                    r0 = (((l * 2 + which) * 16 + dc) * 2 + hh) * 128
                    w, wk = W.get(w2d[r0:r0 + 128, :], 2816)
                    w3 = w[:, 0:2816].rearrange("p (f c) -> p f c", f=22)
                    for sub in range(NS):
                        py = next_ps()
                        mm(PS[py][:], [(w3[:, fc, :], GT[:, fc, ssl(sub)]) for fc in range(22)],
                           [wk] + [("gT", fc, sub) for fc in range(22)], [("ps", py)])
                        stt(XT[0][:, dc, ssl(sub)], PS[py][:], 0.5, XT[0][:, dc, ssl(sub)], ALU.mult, ALU.add,
                            [("ps", py), ("xt", 0, dc, sub)], [("xt", 0, dc, sub)])
            dma(dst[:, :, tsl], XT[0][:], xtk, (), "xst0")
            if final:
                P.barrier()
                YO = alloc("yo", [128, KC, SUB], F32, at=off)
                for sub in range(NS):
                    norm_sub(L * 3, sub, lambda kc: YO[:, kc, :], lambda kc: [("yo", kc)])
                    dma(yout[:, :, t * TT + sub * SUB:t * TT + (sub + 1) * SUB], YO[:], [("yo", k) for k in range(KC)], (), "yo")
                P.barrier()
        P.barrier()

    def m1_stage(l):
        off = cur[0]
        limit[0] = GS_OFF
        NTB = TT // 128
        STF = [alloc(f"stf{i}", [128, 2, TT], BF16) for i in range(2)]
        STT = [alloc(f"stt{i}", [128, NTB, 256], BF16) for i in range(2)]
        RT = [alloc(f"rt{i}", [128, 256], F32) for i in range(2)]
        RU = [alloc(f"ru{i}", [128, 256], F32) for i in range(2)]
        COST = alloc("cost", [128, NTB, 256], F32)
        SINT = alloc("sint", [128, NTB, 256], F32)
        ETMP = alloc("etmp", [128, SUB], F32)
        cur[0] = off
        gi = l * 3 + 1
        fi = [0]
        ti = [0]
        ri = [0]
        fams_f = [(OFF_POOL, US, "pool"), (OFF_FQ, FQS, "fq"), (OFF_FK, FKS, "fk"), (OFF_RG, RGS, "rg")]
        fams_t = [(OFF_FV, FVS, "fv"), (OFF_RV, RVS, "rv"), (OFF_RQ, RQS, "rq"), (OFF_RK, RKS, "rk")]
        for t in range(NT):
            tsl = slice(t * TT, (t + 1) * TT)
            dma(XT[0][:], XS[:, :, tsl], (), xtk, "xld0")
            dma(COST[:], cosd[:, t * NTB:(t + 1) * NTB, :], (), ["cost"], "cld1")
            dma(SINT[:], sind[:, t * NTB:(t + 1) * NTB, :], (), ["sint"], "cld2")
            norm_xn(gi)
            dma(XNS[:, :, tsl], XN[:], xnk_all, (), "xnst")
            for fidx, (coff, scr, nm) in enumerate(fams_f):
                for g in range(4):
                    r0 = ((l * 32) + fidx * 4 + g) * 128
                    w, wk = W.get(wind[r0:r0 + 128, :], 4096)
                    w3 = w[:, 0:4096].rearrange("p (k c) -> p k c", k=KC)
                    r = fi[0] % 2
                    fi[0] += 1
                    for j in range(2):
                        for sub in range(NS):
                            p = next_ps()
                            mm(PS[p][:], [(w3[:, kc, j * 128:(j + 1) * 128], XN[:, kc, ssl(sub)]) for kc in range(KC)],
                               [wk] + xnk(sub), [("ps", p)])
                            dst_ap = STF[r][:, j, ssl(sub)]
                            wkeys = [("stf", r, j, sub)]
                            if nm == "pool":
                                act(dst_ap, PS[p][:], AF.Copy, [("ps", p)], wkeys)
                            elif nm == "fq":
                                act(dst_ap, PS[p][:], AF.Copy, [("ps", p)], wkeys, scale=float(128 ** -0.5))
                            elif nm == "fk":
                                cp(dst_ap, PS[p][:], [("ps", p)], wkeys)
                            else:
                                act(dst_ap, PS[p][:], AF.Silu, [("ps", p)], wkeys)
                    dma(scr[:, g * 2:g * 2 + 2, tsl], STF[r][:], [("stf", r, j, sb) for j in range(2) for sb in range(NS)], (), f"stf{r}")
            for fidx, (coff, scr, nm) in enumerate(fams_t):
                for g in range(4):
                    r0 = ((l * 32) + 16 + fidx * 4 + g) * 128
                    w, wk = W.get(wind[r0:r0 + 128, :], 4096)
                    w3 = w[:, 0:4096].rearrange("p (k c) -> p k c", k=KC)
                    r = ti[0] % 2
                    ti[0] += 1
                    for tb in range(NTB):
                        p = next_ps()
                        mm(PS[p][:, 0:256], [(XN[:, kc, tb * 128:(tb + 1) * 128], w3[:, kc, :]) for kc in range(KC)],
                           [wk] + xnk(tb // 4), [("ps", p)])
                        if nm == "fv":
                            cp(STT[r][:, tb, :], PS[p][:, 0:256], [("ps", p)], [("stt", r, tb)])
                        elif nm == "rv":
                            for hh in range(2):
                                h = g * 2 + hh
                                act(STT[r][:, tb, hh * 128:(hh + 1) * 128], PS[p][:, hh * 128:(hh + 1) * 128], AF.Copy,
                                    [("ps", p)], [("stt", r, tb, hh)], scale=ZETA[:, h:h + 1])
                        else:
                            q = ri[0] % 2
                            ri[0] += 1
                            x4 = PS[p][:, 0:256].rearrange("p (h t d) -> p h t d", h=2, t=2)
                            s4 = SINT[:, tb, :].rearrange("p (h t d) -> p h t d", h=2, t=2)
                            u4 = RU[q][:].rearrange("p (h t d) -> p h t d", h=2, t=2)
                            tt(RT[q][:], PS[p][:, 0:256], COST[:, tb, :], ALU.mult, [("ps", p), "cost"], [("rt", q)])
                            tt(u4[:, :, 0, :], x4[:, :, 1, :], s4[:, :, 0, :], ALU.mult, [("ps", p), "sint"], [("ru", q, 0)])
                            tt(u4[:, :, 1, :], x4[:, :, 0, :], s4[:, :, 1, :], ALU.mult, [("ps", p), "sint"], [("ru", q, 1)])
                            tt(STT[r][:, tb, :], RT[q][:], RU[q][:], ALU.add, [("rt", q), ("ru", q, 0), ("ru", q, 1)],
                               [("stt", r, tb)])
                    rk = [("stt", r, tb) for tb in range(NTB)] + [("stt", r, tb, hh) for tb in range(NTB) for hh in range(2)]
                    dma(scr[:, t * NTB:(t + 1) * NTB, g * 256:(g + 1) * 256], STT[r][:], rk, (), f"stt{r}")
            w, wk = W.get(wfld[l * 128:(l + 1) * 128, :], KC * 72)
            w3 = w[:, 0:KC * 72].rearrange("p (k c) -> p k c", k=KC)
            for sub in range(NS):
                p = next_ps()
                mm(PS[p][0:72, :], [(w3[:, kc, :], XN[:, kc, ssl(sub)]) for kc in range(KC)], [wk] + xnk(sub), [("ps", p)])
                act(ETMP[0:72, :], PS[p][0:72, :], AF.Exp, [("ps", p)], ["etmp"], bias=NFB[0:72, l:l + 1], scale=-1.0)
                c0 = t * TT + sub * SUB
                act(GS[0:72, c0:c0 + SUB], ETMP[0:72, :], AF.Ln, ["etmp"], [("gs", t, sub)], bias=ONE_C[0:72, 0:1], scale=1.0)
        P.barrier()

    def m2_stage(l):
        off = cur[0]
        limit[0] = GS_OFF
        UB = alloc("ub", [128, S], BF16)
        A = alloc("pa", [128, S + 16], F32)
        S1 = alloc("ps1", [128, S + 16], F32)
        S2 = alloc("ps2", [128, S + 16], F32)
        PL = alloc("pl", [128, 2, S], BF16)
        T16 = alloc("t16", [128, 16], F32)
        YST = [alloc(f"yst{i}", [128, SUB], BF16) for i in range(2)]
        cur[0] = off
        memset(A[:, 0:16], 0.0, ["a"])
        memset(S1[:, 0:16], 0.0, ["s1"])
        memset(S2[:, 0:16], 0.0, ["s2"])
        yi = 0
        for g in range(4):
            wdw = 2 ** (g + 1)
            for ic in range(2):
                c = g * 2 + ic
                dma(UB[:], US[:, c, :], (), ["ub"], "uld")
                act(A[:, 16:], UB[:], AF.Copy, ["ub"], ["a"])
                curt, curk = A, "a"
                for k in range(g + 1):
                    sh = 2 ** k
                    dst, dk = (S1, "s1") if k % 2 == 0 else (S2, "s2")
                    tt(dst[:, 16:], curt[:, 16:], curt[:, 16 - sh:S + 16 - sh], ALU.add, [curk], [dk])
                    curt, curk = dst, dk
                stt(PL[:, ic, :], curt[:, 16:], 1.0 / wdw, A[:, 16:], ALU.mult, ALU.subtract, [curk, "a"], [("pl", ic)])
                tt(T16[:], curt[:, 16:32], INVC[:, g, :], ALU.mult, [curk], ["t16"])
                tt(PL[:, ic, 0:16], T16[:], A[:, 16:32], ALU.subtract, ["t16", "a", ("pl", ic)], [("pl", ic)])
            w, wk = W.get(pwd[(l * 4 + g) * 128:(l * 4 + g + 1) * 128, :], 512)
            w3 = w[:, 0:512].rearrange("p (i o) -> p i o", i=2)
            for oc in range(2):
                for th in range(NQ):
                    p = next_ps()
                    mm(PS[p][:], [(w3[:, ic, oc * 128:(oc + 1) * 128], PL[:, ic, th * SUB:(th + 1) * SUB]) for ic in range(2)],
                       [wk, ("pl", 0), ("pl", 1)], [("ps", p)])
                    r = yi % 2
                    yi += 1
                    act(YST[r][:], PS[p][:], AF.Copy, [("ps", p)], [("yst", r)], scale=PSC[:, l, g * 2 + oc:g * 2 + oc + 1])
                    dma(YPS[:, g * 2 + oc, th * SUB:(th + 1) * SUB], YST[r][:], [("yst", r)], (), f"yst{r}")
        P.barrier()

    def m3_stage(l):
        off = cur[0]
        limit[0] = GS_OFF
        GP = alloc("gp", [128, S], F32)
        R1 = alloc("r1", [128, S], F32)
        R2 = alloc("r2", [128, S], F32)
        MID = alloc("mid", [128, S], BF16)
        cur[0] = off
        src, sk, dst, dk = GS, "gs", GP, "gp"
        s = 1
        while s < S:
            tt(dst[0:72, s:], src[0:72, s:], src[0:72, 0:S - s], ALU.add, [sk], [dk])
            cp(dst[0:72, 0:s], src[0:72, 0:s], [sk, dk], [dk])
            src, sk, dst, dk = dst, dk, src, sk
            s *= 2
        G, gk = src, [sk]
        memset(FR[:], 0.0, ["fr"])
        cp(FR[0:72, :], G[0:72, :], gk + ["fr"], ["fr"])
        tt(R1[32:40, :], G[32:40, :], FR[32:40, :], ALU.subtract, gk + ["fr"], ["r1"])
        tt(R1[64:72, :], G[64:72, :], FR[64:72, :], ALU.subtract, gk + ["fr"], ["r1b"])
        cp(MID[32:40, :], R1[32:40, :], ["r1"], ["mid"])
        cp(MID[64:72, :], R1[64:72, :], ["r1b"], ["midb"])
        tt(R2[64:72, :], R1[64:72, :], MID[64:72, :], ALU.subtract, ["r1b", "midb"], ["r2"])
        cp(FR[32:40, :], MID[32:40, :], ["mid", "fr", "r1", "r1b"], ["fr"])
        cp(FR[64:72, :], R2[64:72, :], ["r2", "fr"], ["fr"])
        p = next_ps()
        for kb in range(NB):
            mm(PS[p][:, kb * 8:(kb + 1) * 8], [(G[0:8, kb * 128:(kb + 1) * 128], I8[0:8, 0:8])], gk, [("ps", p)])
        act(GTT[:], PS[p][:, 0:128], AF.Copy, [("ps", p)], ["gtt"])
        P.barrier()
        off = cur[0]
        FQHs = [alloc(f"fqh{i}", [128, S], BF16) for i in range(2)]
        FKHs = [alloc(f"fkh{i}", [128, S], BF16) for i in range(2)]
        FVHs = [alloc(f"fvh{i}", [128, NB, 128], BF16) for i in range(2)]
        PTL = [alloc(f"ptl{i}", [128, SUB], BF16) for i in range(3)]
        RDEN = alloc("rden", [128, SUB], F32)
        YST = [alloc(f"yst{i}", [128, SUB], BF16) for i in range(2)]
        cur[0] = off
        pi = 0
        yi = 0

        def head_loads(h):
            hb = h % 2
            dma(FQHs[hb][:], FQS[:, h, :], (), [("fqh", hb), "hchain"], "hld0")
            dma(FKHs[hb][:], FKS[:, h, :], (), [("fkh", hb), "hchain"], "hld0")
            dma(FVHs[hb][:], FVS[:, :, h * 128:(h + 1) * 128], (), [("fvh", hb), "hchain"], "hld0")

        head_loads(0)
        head_loads(1)
        its = []
        for h in range(H):
            for qg in range(NQ):
                nkb = 4 * qg + 4
                for kb in range(nkb):
                    its.append((h, qg, kb, nkb))
        state = {"pi": 0, "yi": 0}

        def emit_qk(it):
            h, qg, kb, nkb = it
            hb = h % 2
            FQH, FKH = FQHs[hb], FKHs[hb]
            c0 = max(0, kb * 128 - qg * SUB)
            n = SUB - c0
            qs = slice(qg * SUB + c0, (qg + 1) * SUB)
            diag = kb >= 4 * qg
            psn = state["pi"] % 2
            r = state["pi"] % 3
            state["pi"] += 1

            def fn(e, psn=psn, kb=kb, qs=qs, n=n, diag=diag, h=h, FKH=FKH, FQH=FQH):
                e.matmul(PS[psn][:, 0:n], FKH[:, kb * 128:(kb + 1) * 128], FQH[:, qs], start=True, stop=False)
                if diag:
                    e.matmul(PS[psn][:, 0:128], IDENT, NEGM, start=False, stop=False)
                return e.matmul(PS[psn][:, 0:n], SEL[0:72, h, :], FR[0:72, qs], start=False, stop=True)
            P.op("pe", fn, [("fqh", hb), ("fkh", hb), "fr"], [("ps", psn)])
            act(PTL[r][:, 0:n], PS[psn][:, 0:n], AF.Exp, [("ps", psn), "gtt"], [("ptl", r)],
                bias=GTT[:, kb * 8 + h:kb * 8 + h + 1], scale=1.0)
            return (it, r, c0, n)

        def emit_pv(pend):
            (h, qg, kb, nkb), r, c0, n = pend
            hb = h % 2
            FVH = FVHs[hb]
            po, pd = (2, 3) if (h * NQ + qg) % 2 == 0 else (4, 5)

            def fn2(e, po=po, pd=pd, kb=kb, c0=c0, n=n, r=r, nkb=nkb, FVH=FVH):
                e.matmul(PS[po][:, c0:SUB], FVH[:, kb, :], PTL[r][:, 0:n], start=(kb == 0), stop=(kb == nkb - 1))
                return e.matmul(PS[pd][:, c0:SUB], ONESB, PTL[r][:, 0:n], start=(kb == 0), stop=(kb == nkb - 1))
            P.op("pe", fn2, [("ptl", r), ("fvh", hb)], [("ps", po), ("ps", pd)])
            if kb == nkb - 1:
                recip(RDEN[:], PS[pd][:], [("ps", pd)], ["rden"])
                ry = state["yi"] % 2
                state["yi"] += 1
                tt(YST[ry][:], PS[po][:], RDEN[:], ALU.mult, [("ps", po), "rden"], [("yst", ry)])
                dma(YFS[:, h, qg * SUB:(qg + 1) * SUB], YST[ry][:], [("yst", ry)], (), f"yst{ry}")
                if qg == NQ - 1 and h + 2 < H:
                    head_loads(h + 2)

        pend = None
        for it in its:
            nxt = emit_qk(it)
            if pend is not None:
                emit_pv(pend)
            pend = nxt
        emit_pv(pend)
        P.barrier()

    def m3r_stage(l):
        off = cur[0]
        limit[0] = top
        RQH = alloc("rqh", [128, NB, 128], BF16)
        RKH = alloc("rkh", [128, NB, 128], BF16)
        RVH = alloc("rvh", [128, NB, 128], BF16)
        RGH = alloc("rgh", [128, S], BF16)
        RQT = alloc("rqt", [128, S], BF16)
        RQXT = alloc("rqxt", [128, S], BF16)
        RKT = alloc("rkt", [128, S], BF16)
        SDEC = alloc("sdec", [128, NB, 128], BF16)
        STB = alloc("stb", [128, NB, 128], BF16)
        STATE = alloc("state", [128, 128], F32)
        OF = alloc("of", [128, SUB], F32)
        OB = alloc("ob", [128, SUB], BF16)
        OSQ = alloc("osq", [128, SUB], BF16)
        MN = alloc("mn", [128, SUB], F32)
        T1 = alloc("t1", [128, SUB], F32)
        VAR = alloc("var", [128, SUB], F32)
        CC = alloc("cc", [128, SUB], F32)
        YST = [alloc(f"yst{i}", [128, SUB], BF16) for i in range(2)]
        cur[0] = off
        yi = 0
        RLV = int(os.environ.get("MK_R", "9"))
        XV = int(os.environ.get("MK_X", "0"))
        for h in range(H):
            cd = float(np.exp(128.0 * log_gamma(h)))
            hs = slice(h * 128, (h + 1) * 128)
            dma(RQH[:], RQS[:, :, hs], (), ["rqh", "hchain"], "hld0")
            dma(RKH[:], RKS[:, :, hs], (), ["rkh", "hchain"], "hld0")
            dma(RVH[:], RVS[:, :, hs], (), ["rvh", "hchain"], "hld0")
            dma(RGH[:], RGS[:, h, :], (), ["rgh", "hchain"], "hld0")
            if RLV < 1:
                continue
            for grp in range(4):
                gsl = slice(grp * SUB, (grp + 1) * SUB)
                a = next_pt()

                def fnq(e, a=a, grp=grp):
                    ins = None
                    for j in range(4):
                        ins = e.matmul(PS[a][:, j * 128:(j + 1) * 128], RQH[:, grp * 4 + j, :], IDENT, start=True, stop=True)
                    return ins
                P.op("pe", fnq, ["rqh"], [("ps", a)])
                if XV != 2:
                    cp(RQT[:, gsl], PS[a][:], [("ps", a)], [("rqt", grp)])
                for j in range(4 if XV != 1 else 0):
                    n = grp * 4 + j
                    tt(RQXT[:, n * 128:(n + 1) * 128], PS[a][:, j * 128:(j + 1) * 128], XIROW[:, h, :], ALU.mult,
                       [("ps", a)], [("rqxt", n)])
                if XV != 0:
                    continue
                a2 = next_pt()

                def fnk(e, a2=a2, grp=grp):
                    ins = None
                    for j in range(4):
                        ins = e.matmul(PS[a2][:, j * 128:(j + 1) * 128], RKH[:, grp * 4 + j, :], IDENT, start=True, stop=True)
                    return ins
                P.op("pe", fnk, ["rkh"], [("ps", a2)])
                act(RKT[:, gsl], PS[a2][:], AF.Copy, [("ps", a2)], [("rkt", grp)])
            if RLV < 2:
                continue
            for grp in range(4):
                p = grp % 2

                def fns(e, p=p, grp=grp):
                    ins = None
                    for j in range(4):
                        ns = slice((grp * 4 + j) * 128, (grp * 4 + j + 1) * 128)
                        ins = e.matmul(PS[p][:, j * 128:(j + 1) * 128], RKT[:, ns], RQT[:, ns], start=True, stop=True)
                    return ins
                P.op("pe", fns, [("rkt", grp), ("rqt", grp)], [("ps", p)])
                for j in range(4):
                    n = grp * 4 + j
                    tt(SDEC[:, n, :], PS[p][:, j * 128:(j + 1) * 128], DTAB[:, h, :], ALU.mult, [("ps", p)], [("sdec", n)])
            if RLV < 3:
                continue
            memset(STATE[:], 0.0, ["state"])
            kvp = [2, 3, 4, 5]
            for grp in range(4):
                p = kvp[grp]

                def fnkv(e, p=p, grp=grp):
                    ins = None
                    for j in range(4):
                        n = grp * 4 + j
                        ins = e.matmul(PS[p][:, j * 128:(j + 1) * 128], RKH[:, n, :], RVH[:, n, :], start=True, stop=True)
                    return ins
                P.op("pe", fnkv, ["rkh", "rvh"], [("ps", p)])
            for n in range(NB):
                cp(STB[:, n, :], STATE[:], ["state"], [("stb", n)])
                p, j = kvp[n // 4], n % 4
                stt(STATE[:], STATE[:], cd, PS[p][:, j * 128:(j + 1) * 128], ALU.mult, ALU.add,
                    ["state", ("ps", p)], ["state"])
            if RLV < 4:
                continue
            for grp in range(4):
                gsl = slice(grp * SUB, (grp + 1) * SUB)
                po = grp % 2

                def fno(e, po=po, grp=grp):
                    ins = None
                    for j in range(4):
                        n = grp * 4 + j
                        ns = slice(n * 128, (n + 1) * 128)
                        e.matmul(PS[po][:, j * 128:(j + 1) * 128], RVH[:, n, :], SDEC[:, n, :], start=True, stop=False)
                        ins = e.matmul(PS[po][:, j * 128:(j + 1) * 128], STB[:, n, :], RQXT[:, ns], start=False, stop=True)
                    return ins
                P.op("pe", fno, ["rvh"] + [(k, grp * 4 + j) for j in range(4) for k in ("sdec", "stb", "rqxt")], [("ps", po)])
                pok = [("ps", po)]
                act(OF[:], PS[po][:], AF.Copy, pok, ["of"])
                act(OB[:], PS[po][:], AF.Copy, pok, ["ob"])
                act(OSQ[:], PS[po][:], AF.Square, pok, ["osq"])
                if RLV < 5:
                    continue
                pm, pq = 1 - (grp % 2), kvp[grp]
                mm(PS[pm][:], [(ONES128, OB[:])], ["ob"], [("ps", pm)])
                mm(PS[pq][:], [(ONES128, OSQ[:])], ["osq"], [("ps", pq)])
                act(MN[:], PS[pm][:], AF.Copy, [("ps", pm)], ["mn"])
                tt(T1[:], MN[:], MN[:], ALU.mult, ["mn"], ["t1"])
                stt(VAR[:], T1[:], -1.0, PS[pq][:], ALU.mult, ALU.add, ["t1", ("ps", pq)], ["var"])
                ts(VAR[:], VAR[:], 0.0, ALU.max, ["var"], ["var"])
                act(T1[:], VAR[:], AF.Sqrt, ["var", "t1"], ["t1"], bias=EPS_GN[:, 0:1], scale=1.0)
                recip(VAR[:], T1[:], ["t1", "var"], ["var"])
                tt(CC[:], OF[:], MN[:], ALU.subtract, ["of", "mn"], ["cc"])
                tt(CC[:], CC[:], VAR[:], ALU.mult, ["cc", "var"], ["cc"])
                r = yi % 2
                yi += 1
                tt(YST[r][:], CC[:], RGH[:, gsl], ALU.mult, ["cc", "rgh"], [("yst", r)])
                dma(YRS[:, h, gsl], YST[r][:], [("yst", r)], (), f"yst{r}")
        P.barrier()

    def m4_stage(l):
        off = cur[0]
        limit[0] = top
        MG = alloc("mg", [128, KC, TT], BF16)
        SG = [alloc(f"sg{i}", [128, SUB], F32) for i in range(3)]
        MACC = [alloc(f"macc{i}", [128, SUB], F32) for i in range(2)]
        TMP = [alloc(f"tmp{i}", [128, SUB], F32) for i in range(2)]
        cur[0] = off
        YT = [alloc(f"yt{i}", [128, 8, TT], BF16, at=offs["xt0"] + i * 8 * TT * 2) for i in range(3)]
        ysrc = [YPS, YFS, YRS]
        for t in range(NT):
            tsl = slice(t * TT, (t + 1) * TT)
            dma(XN[:], XNS[:, :, tsl], (), xnk_all, "xnld")
            for i in range(3):
                dma(YT[i][:], ysrc[i][:, :, tsl], (), [("yt", i)], f"yld{i}")
            for dc in range(KC):
                r0 = (l * 16 + dc) * 128
                wa, wak = W.get(wgad[r0:r0 + 128, :], 4096)
                wb, wbk = W.get(wgbd[r0:r0 + 128, :], 2048, group=True)
                wr, wrk = W.get(wbrd[r0:r0 + 128, :], 3072, group=True)
                wa3 = wa[:, 0:4096].rearrange("p (k c) -> p k c", k=KC)
                wb3 = wb[:, 0:2048].rearrange("p (k c) -> p k c", k=KC)
                wr4 = wr[:, 0:3072].rearrange("p (b c j) -> p b c j", b=3, c=8)
                for br in range(3):
                    for sub in range(NS):
                        pg, pb = next_ps(), next_ps()
                        if br < 2:
                            mm(PS[pg][:], [(wa3[:, kc, br * 128:(br + 1) * 128], XN[:, kc, ssl(sub)]) for kc in range(KC)],
                               [wak] + xnk(sub), [("ps", pg)])
                        else:
                            mm(PS[pg][:], [(wb3[:, kc, :], XN[:, kc, ssl(sub)]) for kc in range(KC)], [wbk] + xnk(sub), [("ps", pg)])
                        mm(PS[pb][:], [(wr4[:, br, c, :], YT[br][:, c, ssl(sub)]) for c in range(8)], [wrk, ("yt", br)], [("ps", pb)])
                        act(SG[br][:], PS[pg][:], AF.Sigmoid, [("ps", pg)], [("sg", br)])
                        if br == 0:
                            tt(MACC[sub][:], SG[0][:], PS[pb][:], ALU.mult, [("sg", 0), ("ps", pb)], [("macc", sub)])
                        elif br == 1:
                            tt(TMP[0][:], SG[1][:], PS[pb][:], ALU.mult, [("sg", 1), ("ps", pb)], [("tmp", 0)])
                            tt(MACC[sub][:], MACC[sub][:], TMP[0][:], ALU.add, [("macc", sub), ("tmp", 0)], [("macc", sub)])
                        else:
                            tt(TMP[1][:], SG[2][:], PS[pb][:], ALU.mult, [("sg", 2), ("ps", pb)], [("tmp", 1)])
                            tt(MG[:, dc, ssl(sub)], MACC[sub][:], TMP[1][:], ALU.add, [("macc", sub), ("tmp", 1)], [("mg", dc, sub)])
            P.barrier()
            dma(XT[0][:], XS[:, :, tsl], (), xtk, "xld0")
            for dp in range(8):
                r0 = (l * 8 + dp) * 128
                w, wk = W.get(woutd[r0:r0 + 128, :], 4096)
                w3 = w[:, 0:4096].rearrange("p (k c) -> p k c", k=KC)
                for j in range(2):
                    dc = dp * 2 + j
                    for sub in range(NS):
                        p = next_ps()
                        mm(PS[p][:], [(w3[:, kc, j * 128:(j + 1) * 128], MG[:, kc, ssl(sub)]) for kc in range(KC)],
                           [wk] + [("mg", kc, sub) for kc in range(KC)], [("ps", p)])
                        tt(XT[0][:, dc, ssl(sub)], PS[p][:], XT[0][:, dc, ssl(sub)], ALU.add,
                           [("ps", p), ("xt", 0, dc, sub)], [("xt", 0, dc, sub)])
            dma(XS[:, :, tsl], XT[0][:], xtk, (), "xst0")
            P.barrier()

    GS = alloc("gs", [128, S], F32, at=GS_OFF)
    FR = alloc("fr", [128, S], BF16, at=GS_OFF + 8192)
    GTT = alloc("gtt", [128, 128], F32)
    ONE_C = alloc("onec", [128, 2], F32)
    EPS_GN = alloc("epsg", [128, 2], F32)
    ov_base = cur[0]

    def trace():
        W.cons = 0
        W.safe = 0
        psi[0] = 0
        pti[0] = 0
        dma(CF[:], cfd[:, :], (), ["cf"], "cf")
        dma(CB[:], cbd[:, :], (), ["cb"], "cb")
        ts(NFB[:], FBR, -1.0, ALU.mult, ["cf"], ["nfb"])
        memset(EPS_RMS[:], RMS_EPS, ["epsr"])
        memset(EPS_GN[:], GN_EPS, ["epsg"])
        memset(ONE_C[:], 1.0, ["onec"])
        P.barrier()
        st = os.environ.get("MK_STAGES", "f0,m1,m2,m3,m3r,m4,f1").split(",")
        for l in range(L):
            if "f0" in st:
                ffn_stage(l, 0, xin if l == 0 else XS, XS, False)
            if "m1" in st:
                m1_stage(l)
            if "m2" in st:
                m2_stage(l)
            if "m3" in st:
                m3_stage(l)
            if "m3r" in st:
                m3r_stage(l)
            if "m4" in st:
                m4_stage(l)
            if "f1" in st:
                ffn_stage(l, 1, XS, xout if l == L - 1 else XS, l == L - 1)

    P.dry = True
    trace()
    P.dry = False
    trace()
    P.barrier()
    P.emit()
    return nc


def _consts(L):
    NG = L * 3 + 1
    lg = np.array([np.log1p(-np.power(2.0, -5.0 - h)) for h in range(H)], np.float64)
    p = np.arange(128, dtype=np.float64)
    zeta = (128.0 ** -0.5) * np.exp((127.0 - p)[:, None] * lg[None, :])
    i8 = np.zeros((128, 8)); i8[:8, :8] = np.eye(8)
    q = np.arange(128, dtype=np.float64)
    dtab = np.zeros((128, 8, 128))
    for h in range(H):
        m = (p[:, None] <= q[None, :])
        dtab[:, h, :] = np.where(m, np.exp((q[None, :] - 127.0) * lg[h]), 0.0)
    xirow = np.zeros((128, 8, 128))
    for h in range(H):
        xirow[:, h, :] = np.exp((q + 1.0) * lg[h])[None, :]
    invc = np.zeros((128, 4, 16))
    for g in range(4):
        w = 2 ** (g + 1)
        invc[:, g, :] = 1.0 / np.minimum(np.arange(16) + 1, w)[None, :]
    tail = np.concatenate([zeta, i8, dtab.reshape(128, -1), xirow.reshape(128, -1), invc.reshape(128, -1)], axis=1)
    cb = np.zeros((128, 4 * 128 + 8 * 128), np.float32)
    cb[:, 0:128] = 1.0
    cb[:, 128:256] = 1.0 / 128.0
    cb[:, 256:384] = np.eye(128)
    cb[:, 384:512] = np.where(p[:, None] > q[None, :], -30000.0, 0.0)
    sel = np.zeros((128, 8, 128), np.float32)
    for h in range(H):
        for r in (h, 32 + h, 64 + h):
            sel[r, h, :] = -1.0
    cb[:, 512:] = sel.reshape(128, -1)
    half = 64
    inv_freq = (np.float32(10000.0) ** (-np.arange(half, dtype=np.float32) / np.float32(half))).astype(np.float32)
    pos = np.arange(S, dtype=np.float32)
    ang = (pos[:, None] * inv_freq[None, :]).astype(np.float32).astype(np.float64)
    cos, sin = np.cos(ang), np.sin(ang)
    cos2 = np.concatenate([cos, cos], axis=1)
    sins = np.concatenate([-sin, sin], axis=1)
    cosr = np.tile(cos2.reshape(NB, 128, 1, 128), (1, 1, 2, 1)).transpose(1, 0, 2, 3).reshape(128, NB, 256)
    sinr = np.tile(sins.reshape(NB, 128, 1, 128), (1, 1, 2, 1)).transpose(1, 0, 2, 3).reshape(128, NB, 256)
    return tail.astype(np.float32), cb.astype(ml_dtypes.bfloat16), np.ascontiguousarray(cosr, np.float32), np.ascontiguousarray(sinr, np.float32)


def _fm(v):
    return v.reshape(v.shape[:-1] + (KC, 128))


def _layout_weights(inp, ls):
    L = len(ls)
    w13 = np.empty((L, 2, FC, 128, KC, 2, 128), np.float32)
    w2 = np.empty((L, 2, 16, 2, 128, 22, 128), np.float32)
    for i, l in enumerate(ls):
        for wi, (a, b) in enumerate((("ffn1_w13", "ffn1_w2"), ("ffn2_w13", "ffn2_w2"))):
            w13[i, wi] = inp[a][l].reshape(KC, 128, 2, FC, 128).transpose(3, 1, 0, 2, 4)
            w2[i, wi] = inp[b][l].reshape(2, 22, 128, 16, 128).transpose(3, 0, 2, 1, 4)
    win = np.empty((L, 32, 128, KC, 256), np.float32)
    wfl = np.zeros((L, 128, KC, 72), np.float32)
    wga = np.empty((L, 16, 128, KC, 2, 128), np.float32)
    wgb = np.empty((L, 16, 128, KC, 128), np.float32)
    wbr = np.empty((L, 16, 128, 3, 8, 128), np.float32)
    wout = np.empty((L, 8, 128, KC, 256), np.float32)
    pw = np.empty((L, 4, 128, 2, 256), np.float32)
    order = [OFF_POOL, OFF_FQ, OFF_FK, OFF_RG, OFF_FV, OFF_RV, OFF_RQ, OFF_RK]
    for i, l in enumerate(ls):
        wi_ = inp["w_in"][l]
        for fi, off in enumerate(order):
            win[i, fi * 4:(fi + 1) * 4] = wi_[:, off:off + 1024].reshape(KC, 128, 4, 256).transpose(2, 1, 0, 3)
        fl = wi_[:, OFF_FL:OFF_FL + 8].reshape(KC, 128, 8).transpose(1, 0, 2)
        for r in (0, 32, 64):
            wfl[i, :, :, r:r + 8] = fl
        gts = wi_[:, OFF_GATE:OFF_GATE + 3 * D]
        wga[i] = gts[:, 0:2 * D].reshape(KC, 128, 2, 16, 128).transpose(3, 1, 0, 2, 4)
        wgb[i] = gts[:, 2 * D:3 * D].reshape(KC, 128, 16, 128).transpose(2, 1, 0, 3)
        br = np.stack([inp["w_branch_pool"][l], inp["w_branch_fox"][l], inp["w_branch_ret"][l]], 0)
        wbr[i] = br.reshape(3, 8, 128, 16, 128).transpose(3, 2, 0, 1, 4)
        wout[i] = inp["w_out"][l].reshape(KC, 128, 8, 256).transpose(2, 1, 0, 3)
        pw[i] = inp["pool_w"][l].reshape(4, 2, 128, 256).transpose(0, 2, 1, 3)
    NG = L * 3 + 1
    gains = np.zeros((NG, D), np.float32)
    for i, l in enumerate(ls):
        gains[i * 3 + 0] = inp["ffn1_norm"][l]
        gains[i * 3 + 1] = inp["mix_norm"][l]
        gains[i * 3 + 2] = inp["ffn2_norm"][l]
    gains[L * 3] = inp["final_norm"]
    gain_fm = gains.reshape(NG, KC, 128).transpose(2, 0, 1).reshape(128, NG * KC)
    psc = np.stack([inp["pool_scale"][l] for l in ls], 0).reshape(L, 8, 128).transpose(2, 0, 1).reshape(128, L * 8)
    fbr = np.zeros((128, L), np.float32)
    for i, l in enumerate(ls):
        for r in (0, 32, 64):
            fbr[r:r + 8, i] = inp["forget_bias"][l]
    tail, cb, cosr, sinr = _consts(L)
    cf = np.ascontiguousarray(np.concatenate([gain_fm, psc, fbr, tail], axis=1), np.float32)
    return {
        "w13": w13.reshape(-1, 4096), "w2": w2.reshape(-1, 2816), "win": win.reshape(-1, 4096),
        "wfl": wfl.reshape(-1, KC * 72), "wga": wga.reshape(-1, 4096), "wgb": wgb.reshape(-1, 2048),
        "wbr": wbr.reshape(-1, 3072), "wout": wout.reshape(-1, 4096), "pw": pw.reshape(-1, 512),
        "cf": cf, "cb": cb, "cosr": cosr, "sinr": sinr,
    }


_NC_CACHE = {}


def _get_nc(nl):
    if nl not in _NC_CACHE:
        _NC_CACHE[nl] = build(nl)
    return _NC_CACHE[nl]


LAYERS_PER_LAUNCH = int(os.environ.get("MK_LPL", "4"))
N_LAYERS = int(os.environ.get("MK_NL", str(DEPTH)))


def kernel(**inp):
    inp = {k: np.asarray(v) for k, v in inp.items()}
    B = inp["x"].shape[0]
    xs = [np.ascontiguousarray(inp["x"][b].T.reshape(KC, 128, S).transpose(1, 0, 2)) for b in range(B)]
    ys = None
    l0 = 0
    while l0 < N_LAYERS:
        ls = list(range(l0, min(N_LAYERS, l0 + LAYERS_PER_LAUNCH)))
        nc = _get_nc(len(ls))
        wl = _layout_weights(inp, ls)
        in_maps = [dict(wl, xin=xs[b]) for b in range(B)]
        res = run_bass_kernel_spmd(nc, in_maps, core_ids=list(range(B)))
        xs = [np.ascontiguousarray(res.results[b]["xout"]) for b in range(B)]
        if os.environ.get("MK_DEBUG") == "1":
            global LAST_RES
            LAST_RES = res.results
        ys = [res.results[b]["yout"] for b in range(B)]
        l0 += len(ls)
    out = np.stack([y.transpose(1, 0, 2).reshape(D, S).T for y in ys], 0)
    return np.ascontiguousarray(out, np.float32)
```

```python
import os
import numpy as np
import ml_dtypes
import concourse.bass as bass
import concourse.mybir as mybir
from concourse.bass_utils import run_bass_kernel_spmd

F32 = mybir.dt.float32
BF16 = mybir.dt.bfloat16
ALU = mybir.AluOpType
AF = mybir.ActivationFunctionType

D = 2048
S = 2048
KC = 16
TT = 1024
NT = S // TT
SUB = 512
NS = TT // SUB
NQ = S // SUB
DFF = 5632
FC = 44
H = 8
NB = S // 128
DEPTH = 4
RMS_EPS = 1e-6
GN_EPS = 1e-5
OFF_POOL, OFF_FQ, OFF_FK, OFF_FV, OFF_FL, OFF_RQ, OFF_RK, OFF_RV, OFF_RG, OFF_GATE = (
    0, 1024, 2048, 3072, 4096, 4104, 5128, 6152, 7176, 8200)
NSLOT = 5
STRICT = int(os.environ.get("MK_STRICT", "1"))
SLOT_ELEMS = 4096


class Op:
    __slots__ = ("eng", "fn", "deps", "dsem", "signal", "sem", "inc", "waits", "val")

    def __init__(self, eng, fn, deps, dsem):
        self.eng, self.fn, self.deps, self.dsem = eng, fn, deps, dsem
        self.signal = dsem is not None
        self.sem = None
        self.inc = 0
        self.val = 0
        self.waits = []


class Prog:
    ENGS = ("pe", "act", "dve", "pool", "sp")

    def __init__(self, nc):
        self.nc = nc
        self.ops = []
        self.lw = {}
        self.rd = {}
        self.dry = False
        self.last_eng = {}
        self.last_dma = {}

    def op(self, eng, fn, reads=(), writes=(), dsem=None):
        if self.dry:
            return
        i = len(self.ops)
        deps = set()
        for k in reads:
            j = self.lw.get(k)
            if j is not None:
                deps.add(j)
        for k in writes:
            j = self.lw.get(k)
            if j is not None:
                deps.add(j)
            deps.update(self.rd.get(k, ()))
        for k in reads:
            self.rd.setdefault(k, []).append(i)
        for k in writes:
            self.lw[k] = i
            self.rd[k] = []
        self.ops.append(Op(eng, fn, deps, dsem))
        if dsem is None:
            self.last_eng[eng] = i
        else:
            self.last_dma[dsem] = i

    def barrier(self):
        if self.dry:
            return
        deps = set(self.last_eng.values()) | set(self.last_dma.values())
        for e in self.ENGS:
            self.ops.append(Op(e, None, set(deps), None))
        self.lw = {}
        self.rd = {}

    def finalize(self):
        nc = self.nc
        ops = self.ops
        for o in ops:
            for j in o.deps:
                d = ops[j]
                if d.dsem is None and (STRICT or d.eng != o.eng):
                    d.signal = True
        esem = {e: nc.alloc_semaphore("s_" + e) for e in self.ENGS}
        dsems = {}
        ecnt = {e: 0 for e in self.ENGS}
        dcnt = {}
        for o in ops:
            if o.dsem is not None:
                if o.dsem not in dsems:
                    dsems[o.dsem] = nc.alloc_semaphore("d_" + o.dsem)
                    dcnt[o.dsem] = 0
                dcnt[o.dsem] += 16
                o.sem, o.inc, o.val = dsems[o.dsem], 16, dcnt[o.dsem]
            elif o.signal:
                ecnt[o.eng] += 1
                o.sem, o.inc, o.val = esem[o.eng], 1, ecnt[o.eng]
        waited = {e: {} for e in self.ENGS}
        for o in ops:
            need = {}
            for j in o.deps:
                d = ops[j]
                if d.dsem is None and d.eng == o.eng and (not STRICT or o.eng == "pe"):
                    continue
                key = id(d.sem)
                if key not in need or need[key][1] < d.val:
                    need[key] = (d.sem, d.val)
            w = waited[o.eng]
            for key, (sem, val) in need.items():
                if w.get(key, 0) >= val:
                    continue
                w[key] = val
                o.waits.append((sem, val))
        self.by_eng = {e: [o for o in ops if o.eng == e] for e in self.ENGS}
        if os.environ.get("MK_VERBOSE"):
            print("sem counts", ecnt, "dma", {k: v for k, v in dcnt.items()}, "nops", len(ops), flush=True)

    def emit(self):
        nc = self.nc
        self.finalize()

        def mk(name):
            def body(e):
                for o in self.by_eng[name]:
                    for sem, val in o.waits:
                        e.wait_ge(sem, val)
                    if o.fn is not None:
                        ins = o.fn(e)
                        if o.signal:
                            ins.then_inc(o.sem, o.inc)
            return body

        with nc.Block() as block:
            block.tensor(mk("pe"))
            block.scalar(mk("act"))
            block.vector(mk("dve"))
            block.gpsimd(mk("pool"))
            block.sync(mk("sp"))


def log_gamma(h):
    return float(np.log1p(-np.power(2.0, -5.0 - h)))


def build(nlayers):
    nc = bass.Bass("TRN2", target_bir_lowering=False)
    P = Prog(nc)
    L = nlayers

    def din(name, shape, dt=F32):
        return nc.dram_tensor(name, shape, dt, kind="ExternalInput").ap()

    xin = din("xin", [128, KC, S])
    w13d = din("w13", [L * 2 * FC * 128, 4096])
    w2d = din("w2", [L * 2 * 32 * 128, 2816])
    wind = din("win", [L * 32 * 128, 4096])
    wfld = din("wfl", [L * 128, KC * 72])
    wgad = din("wga", [L * 16 * 128, 4096])
    wgbd = din("wgb", [L * 16 * 128, 2048])
    wbrd = din("wbr", [L * 16 * 128, 3072])
    woutd = din("wout", [L * 8 * 128, 4096])
    pwd = din("pw", [L * 4 * 128, 512])
    NG = L * 3 + 1
    NCF = NG * 16 + L * 8 + L + 8 + 8 + 8 * 128 + 8 * 128 + 64
    cfd = din("cf", [128, NCF])
    NCB = 4 * 128 + 8 * 128
    cbd = din("cb", [128, NCB], BF16)
    cosd = din("cosr", [128, NB, 256])
    sind = din("sinr", [128, NB, 256])
    xout = nc.dram_tensor("xout", [128, KC, S], F32, kind="ExternalOutput").ap()
    yout = nc.dram_tensor("yout", [128, KC, S], F32, kind="ExternalOutput").ap()

    DBG = os.environ.get("MK_DEBUG") == "1"

    def dscr(name, shape, dt=BF16):
        if DBG and name != "XS":
            return nc.dram_tensor(name, shape, dt, kind="ExternalOutput").ap()
        return nc.dram_tensor(name, shape, dt).ap()

    XS = dscr("XS", [128, KC, S], F32)
    XNS = dscr("XNS", [128, KC, S])
    US = dscr("US", [128, 8, S])
    FQS = dscr("FQS", [128, 8, S])
    FKS = dscr("FKS", [128, 8, S])
    RGS = dscr("RGS", [128, 8, S])
    FVS = dscr("FVS", [128, NB, 1024])
    RVS = dscr("RVS", [128, NB, 1024])
    RQS = dscr("RQS", [128, NB, 1024])
    RKS = dscr("RKS", [128, NB, 1024])
    YPS = dscr("YPS", [128, 8, S])
    YFS = dscr("YFS", [128, 8, S])
    YRS = dscr("YRS", [128, 8, S])

    base = (nc.sbuf_base + 63) // 64 * 64
    top = nc.sbuf_top
    cur = [base]

    uid = [0]
    offs = {}
    limit = [top]
    GS_OFF = (top - 8192 - 4096 - 128) // 64 * 64

    def alloc(name, shape, dt, at=None):
        uid[0] += 1
        name = f"{name}_{uid[0]}"
        nbytes = int(np.prod(shape[1:])) * (4 if dt == F32 else 2)
        if at is None:
            off = cur[0]
            cur[0] = (off + nbytes + 63) // 64 * 64
            assert off + nbytes <= limit[0], (name, off, nbytes, limit[0])
        else:
            off = at
        assert off + nbytes <= top, (name, off, nbytes, top)
        offs[name.rsplit("_", 1)[0]] = off
        return nc.alloc_sbuf_tensor_at(name, list(shape), dt, offset=off)

    WSL = [alloc(f"ws{i}", [128, SLOT_ELEMS], BF16) for i in range(NSLOT)]
    XT = [alloc("xt0", [128, KC, TT], F32)]
    XN = alloc("xn", [128, KC, TT], BF16)
    CF = alloc("cf", [128, NCF], F32)
    CB = alloc("cb", [128, NCB], BF16)
    NFB = alloc("nfb", [128, L], F32)
    SQ = [alloc(f"sq{i}", [128, SUB], BF16) for i in range(2)]
    RMS = alloc("rms", [128, SUB], F32)
    RSTD = alloc("rstd", [128, SUB], F32)
    ov_base = cur[0]

    o = 0
    GAIN = CF[:, o:o + NG * 16].rearrange("p (g k) -> p g k", k=16); o += NG * 16
    PSC = CF[:, o:o + L * 8].rearrange("p (l c) -> p l c", c=8); o += L * 8
    FBR = CF[:, o:o + L]; o += L
    ZETA = CF[:, o:o + 8]; o += 8
    I8 = CF[:, o:o + 8]; o += 8
    DTAB = CF[:, o:o + 1024].rearrange("p (h q) -> p h q", h=8); o += 1024
    XIROW = CF[:, o:o + 1024].rearrange("p (h q) -> p h q", h=8); o += 1024
    INVC = CF[:, o:o + 64].rearrange("p (g j) -> p g j", g=4); o += 64
    ONESB = CB[:, 0:128]
    ONES128 = CB[:, 128:256]
    IDENT = CB[:, 256:384]
    NEGM = CB[:, 384:512]
    SEL = CB[:, 512:512 + 1024].rearrange("p (h m) -> p h m", h=8)

    PS = [nc.alloc_psum_tensor(f"ps{i}", [128, 512], F32) for i in range(6)]
    psi = [0]
    pti = [0]

    def next_ps():
        i = psi[0] % 6
        psi[0] += 1
        return i

    def next_pt():
        i = 2 + pti[0] % 4
        pti[0] += 1
        return i

    class WStream:
        def __init__(self):
            self.reqs = []
            self.cons = 0
            self.issued = 0
            self.safe = 0

        def get(self, src, n, group=False):
            j = self.cons
            self.cons += 1
            if not group:
                self.safe = j
            if P.dry:
                self.reqs.append((src, n))
            else:
                while self.issued < min(len(self.reqs), j + NSLOT) and self.issued - NSLOT < self.safe:
                    i = self.issued
                    s = i % NSLOT
                    src_i, n_i = self.reqs[i]
                    P.op("pool", lambda e, s=s, src_i=src_i, n_i=n_i: e.dma_start(out=WSL[s][:, 0:n_i], in_=src_i),
                         reads=(), writes=[("ws", s)], dsem=f"ws{s}")
                    self.issued += 1
            return WSL[j % NSLOT], ("ws", j % NSLOT)

    W = WStream()

    def mm(out, pairs, reads, writes, start=True, stop=True):
        def fn(e, out=out, pairs=pairs, start=start, stop=stop):
            n = len(pairs)
            ins = None
            for i, (l, r) in enumerate(pairs):
                ins = e.matmul(out, l, r, start=(start and i == 0), stop=(stop and i == n - 1))
            return ins
        P.op("pe", fn, reads, writes)

    def act(out, in_, func, reads, writes, bias=None, scale=None):
        def fn(e):
            kw = {}
            if bias is not None:
                kw["bias"] = bias
            if scale is not None:
                kw["scale"] = scale
            return e.activation(out=out, in_=in_, func=func, **kw)
        P.op("act", fn, reads, writes)

    def tt(out, a, b, op, reads, writes, eng="dve"):
        P.op(eng, lambda e: e.tensor_tensor(out=out, in0=a, in1=b, op=op), reads, writes)

    def stt(out, a, sc, b, op0, op1, reads, writes, eng="dve"):
        P.op(eng, lambda e: e.scalar_tensor_tensor(out=out, in0=a, scalar=sc, in1=b, op0=op0, op1=op1), reads, writes)

    def ts(out, a, s1, op0, reads, writes, s2=None, op1=None, eng="dve"):
        def fn(e):
            if op1 is None:
                return e.tensor_scalar(out=out, in0=a, scalar1=s1, scalar2=None, op0=op0)
            return e.tensor_scalar(out=out, in0=a, scalar1=s1, scalar2=s2, op0=op0, op1=op1)
        P.op(eng, fn, reads, writes)

    def cp(out, in_, reads, writes, eng="dve"):
        P.op(eng, lambda e: e.tensor_copy(out=out, in_=in_), reads, writes)

    def recip(out, in_, reads, writes):
        P.op("dve", lambda e: e.reciprocal(out=out, in_=in_), reads, writes)

    def memset(ap, v, writes, eng="dve"):
        P.op(eng, lambda e: e.memset(ap, v), (), writes)

    def dma(out, in_, reads, writes, dsem, eng="sp"):
        P.op(eng, lambda e: e.dma_start(out=out, in_=in_), reads, writes, dsem=dsem)

    xtk = [("xt", 0, k, sb) for k in range(KC) for sb in range(NS)]
    xnk_all = [("xn", k, sb) for k in range(KC) for sb in range(NS)]

    def xnk(sub):
        return [("xn", k, sub) for k in range(KC)]

    def ssl(sub):
        return slice(sub * SUB, (sub + 1) * SUB)

    def xtks(sub):
        return [("xt", 0, k, sub) for k in range(KC)]

    def load_x(src, t):
        for sub in range(NS):
            c0 = t * TT + sub * SUB
            dma(XT[0][:, :, ssl(sub)], src[:, :, c0:c0 + SUB], (), xtks(sub), f"xld{sub}")

    def store_x(dst, t):
        for sub in range(NS):
            c0 = t * TT + sub * SUB
            dma(dst[:, :, c0:c0 + SUB], XT[0][:, :, ssl(sub)], xtks(sub), (), f"xst{sub}")

    def norm_sub(gi, sub, out_fn, out_keys_fn):
        pss = next_ps()
        for kc in range(KC):
            act(SQ[kc % 2][:], XT[0][:, kc, ssl(sub)], AF.Square, [("xt", 0, kc, sub)], [("sq", kc % 2)])
            mm(PS[pss][:], [(ONESB, SQ[kc % 2][:])], [("sq", kc % 2)], [("ps", pss)], start=(kc == 0), stop=(kc == KC - 1))
        act(RMS[:], PS[pss][:], AF.Sqrt, [("ps", pss)], ["rms"], bias=EPS_RMS[:, 0:1], scale=1.0 / D)
        recip(RSTD[:], RMS[:], ["rms"], ["rstd"])
        for kc in range(KC):
            stt(out_fn(kc), XT[0][:, kc, ssl(sub)], GAIN[:, gi, kc:kc + 1], RSTD[:], ALU.mult, ALU.mult,
                [("xt", 0, kc, sub), "rstd"], out_keys_fn(kc))

    def norm_xn(gi):
        for sub in range(NS):
            norm_sub(gi, sub, lambda kc, sub=sub: XN[:, kc, ssl(sub)], lambda kc, sub=sub: [("xn", kc, sub)])

    EPS_RMS = alloc("epsr", [128, 2], F32)

    def ffn_stage(l, which, src, dst, final):
        off = cur[0]
        limit[0] = top
        GT = alloc("gT", [128, 22, TT], BF16)
        SA = [alloc(f"sa{i}", [128, SUB], F32) for i in range(2)]
        cur[0] = off
        gi = l * 3 + (0 if which == 0 else 2)
        si = 0
        for t in range(NT):
            tsl = slice(t * TT, (t + 1) * TT)
            load_x(src, t)
            norm_xn(gi)
            for hh in range(2):
                for f in range(22):
                    fg = hh * 22 + f
                    r0 = ((l * 2 + which) * FC + fg) * 128
                    w, wk = W.get(w13d[r0:r0 + 128, :], 4096)
                    w3 = w[:, 0:4096].rearrange("p (k c) -> p k c", k=KC)
                    for sub in range(NS):
                        pa, pb = next_ps(), next_ps()
                        mm(PS[pa][:], [(w3[:, kc, 0:128], XN[:, kc, ssl(sub)]) for kc in range(KC)], [wk] + xnk(sub), [("ps", pa)])
                        mm(PS[pb][:], [(w3[:, kc, 128:256], XN[:, kc, ssl(sub)]) for kc in range(KC)], [wk] + xnk(sub), [("ps", pb)])
                        r = si % 2
                        si += 1
                        act(SA[r][:], PS[pa][:], AF.Silu, [("ps", pa)], [("sa", r)])
                        tt(GT[:, f, ssl(sub)], SA[r][:], PS[pb][:], ALU.mult, [("sa", r), ("ps", pb)], [("gT", f, sub)])
                for dc in range(KC):
                    r0 = (((l * 2 + which) * 16 + dc) * 2 + hh) * 128
                    w, wk = W.get(w2d[r0:r0 + 128, :], 2816)
                    w3 = w[:, 0:2816].rearrange("p (f c) -> p f c", f=22)
                    for sub in range(NS):
                        py = next_ps()
                        mm(PS[py][:], [(w3[:, fc, :], GT[:, fc, ssl(sub)]) for fc in range(22)],
                           [wk] + [("gT", fc, sub) for fc in range(22)], [("ps", py)])
                        stt(XT[0][:, dc, ssl(sub)], PS[py][:], 0.5, XT[0][:, dc, ssl(sub)], ALU.mult, ALU.add,
                            [("ps", py), ("xt", 0, dc, sub)], [("xt", 0, dc, sub)])
            store_x(dst, t)
            if final:
                P.barrier()
                YO = alloc("yo", [128, KC, SUB], F32, at=off)
                for sub in range(NS):
                    norm_sub(L * 3, sub, lambda kc: YO[:, kc, :], lambda kc: [("yo", kc)])
                    dma(yout[:, :, t * TT + sub * SUB:t * TT + (sub + 1) * SUB], YO[:], [("yo", k) for k in range(KC)], (), "yo")
                P.barrier()
        P.barrier()

    def m1_stage(l):
        off = cur[0]
        limit[0] = GS_OFF
        NTB = TT // 128
        STF = [alloc(f"stf{i}", [128, 2, TT], BF16) for i in range(2)]
        STT = [alloc(f"stt{i}", [128, NTB, 256], BF16) for i in range(2)]
        RT = [alloc(f"rt{i}", [128, 256], F32) for i in range(2)]
        RU = [alloc(f"ru{i}", [128, 256], F32) for i in range(2)]
        COST = alloc("cost", [128, NTB, 256], F32)
        SINT = alloc("sint", [128, NTB, 256], F32)
        ETMP = alloc("etmp", [128, SUB], F32)
        cur[0] = off
        gi = l * 3 + 1
        fi = [0]
        ti = [0]
        ri = [0]
        fams_f = [(OFF_POOL, US, "pool"), (OFF_FQ, FQS, "fq"), (OFF_FK, FKS, "fk"), (OFF_RG, RGS, "rg")]
        fams_t = [(OFF_FV, FVS, "fv"), (OFF_RV, RVS, "rv"), (OFF_RQ, RQS, "rq"), (OFF_RK, RKS, "rk")]
        for t in range(NT):
            tsl = slice(t * TT, (t + 1) * TT)
            load_x(XS, t)
            dma(COST[:], cosd[:, t * NTB:(t + 1) * NTB, :], (), ["cost"], "cld1")
            dma(SINT[:], sind[:, t * NTB:(t + 1) * NTB, :], (), ["sint"], "cld2")
            norm_xn(gi)
            dma(XNS[:, :, tsl], XN[:], xnk_all, (), "xnst")
            for fidx, (coff, scr, nm) in enumerate(fams_f):
                for g in range(4):
                    r0 = ((l * 32) + fidx * 4 + g) * 128
                    w, wk = W.get(wind[r0:r0 + 128, :], 4096)
                    w3 = w[:, 0:4096].rearrange("p (k c) -> p k c", k=KC)
                    r = fi[0] % 2
                    fi[0] += 1
                    for j in range(2):
                        for sub in range(NS):
                            p = next_ps()
                            mm(PS[p][:], [(w3[:, kc, j * 128:(j + 1) * 128], XN[:, kc, ssl(sub)]) for kc in range(KC)],
                               [wk] + xnk(sub), [("ps", p)])
                            dst_ap = STF[r][:, j, ssl(sub)]
                            wkeys = [("stf", r, j, sub)]
                            if nm == "pool":
                                act(dst_ap, PS[p][:], AF.Copy, [("ps", p)], wkeys)
                            elif nm == "fq":
                                act(dst_ap, PS[p][:], AF.Copy, [("ps", p)], wkeys, scale=float(128 ** -0.5))
                            elif nm == "fk":
                                cp(dst_ap, PS[p][:], [("ps", p)], wkeys)
                            else:
                                act(dst_ap, PS[p][:], AF.Silu, [("ps", p)], wkeys)
                    dma(scr[:, g * 2:g * 2 + 2, tsl], STF[r][:], [("stf", r, j, sb) for j in range(2) for sb in range(NS)], (), f"stf{r}")
            for fidx, (coff, scr, nm) in enumerate(fams_t):
                for g in range(4):
                    r0 = ((l * 32) + 16 + fidx * 4 + g) * 128
                    w, wk = W.get(wind[r0:r0 + 128, :], 4096)
                    w3 = w[:, 0:4096].rearrange("p (k c) -> p k c", k=KC)
                    r = ti[0] % 2
                    ti[0] += 1
                    for tb in range(NTB):
                        p = next_ps()
                        mm(PS[p][:, 0:256], [(XN[:, kc, tb * 128:(tb + 1) * 128], w3[:, kc, :]) for kc in range(KC)],
                           [wk] + xnk(tb // 4), [("ps", p)])
                        if nm == "fv":
                            cp(STT[r][:, tb, :], PS[p][:, 0:256], [("ps", p)], [("stt", r, tb)])
                        elif nm == "rv":
                            for hh in range(2):
                                h = g * 2 + hh
                                act(STT[r][:, tb, hh * 128:(hh + 1) * 128], PS[p][:, hh * 128:(hh + 1) * 128], AF.Copy,
                                    [("ps", p)], [("stt", r, tb, hh)], scale=ZETA[:, h:h + 1])
                        else:
                            q = ri[0] % 2
                            ri[0] += 1
                            x4 = PS[p][:, 0:256].rearrange("p (h t d) -> p h t d", h=2, t=2)
                            s4 = SINT[:, tb, :].rearrange("p (h t d) -> p h t d", h=2, t=2)
                            u4 = RU[q][:].rearrange("p (h t d) -> p h t d", h=2, t=2)
                            tt(RT[q][:], PS[p][:, 0:256], COST[:, tb, :], ALU.mult, [("ps", p), "cost"], [("rt", q)])
                            tt(u4[:, :, 0, :], x4[:, :, 1, :], s4[:, :, 0, :], ALU.mult, [("ps", p), "sint"], [("ru", q, 0)])
                            tt(u4[:, :, 1, :], x4[:, :, 0, :], s4[:, :, 1, :], ALU.mult, [("ps", p), "sint"], [("ru", q, 1)])
                            tt(STT[r][:, tb, :], RT[q][:], RU[q][:], ALU.add, [("rt", q), ("ru", q, 0), ("ru", q, 1)],
                               [("stt", r, tb)])
                    rk = [("stt", r, tb) for tb in range(NTB)] + [("stt", r, tb, hh) for tb in range(NTB) for hh in range(2)]
                    dma(scr[:, t * NTB:(t + 1) * NTB, g * 256:(g + 1) * 256], STT[r][:], rk, (), f"stt{r}")
            w, wk = W.get(wfld[l * 128:(l + 1) * 128, :], KC * 72)
            w3 = w[:, 0:KC * 72].rearrange("p (k c) -> p k c", k=KC)
            for sub in range(NS):
                p = next_ps()
                mm(PS[p][0:72, :], [(w3[:, kc, :], XN[:, kc, ssl(sub)]) for kc in range(KC)], [wk] + xnk(sub), [("ps", p)])
                act(ETMP[0:72, :], PS[p][0:72, :], AF.Exp, [("ps", p)], ["etmp"], bias=NFB[0:72, l:l + 1], scale=-1.0)
                c0 = t * TT + sub * SUB
                act(GS[0:72, c0:c0 + SUB], ETMP[0:72, :], AF.Ln, ["etmp"], [("gs", t, sub)], bias=ONE_C[0:72, 0:1], scale=1.0)
        P.barrier()

    def m2_stage(l):
        off = cur[0]
        limit[0] = GS_OFF
        UB = alloc("ub", [128, S], BF16)
        A = alloc("pa", [128, S + 16], F32)
        S1 = alloc("ps1", [128, S + 16], F32)
        S2 = alloc("ps2", [128, S + 16], F32)
        PL = alloc("pl", [128, 2, S], BF16)
        T16 = alloc("t16", [128, 16], F32)
        YST = [alloc(f"yst{i}", [128, SUB], BF16) for i in range(2)]
        cur[0] = off
        memset(A[:, 0:16], 0.0, ["a"])
        memset(S1[:, 0:16], 0.0, ["s1"])
        memset(S2[:, 0:16], 0.0, ["s2"])
        yi = 0
        for g in range(4):
            wdw = 2 ** (g + 1)
            for ic in range(2):
                c = g * 2 + ic
                dma(UB[:], US[:, c, :], (), ["ub"], "uld")
                act(A[:, 16:], UB[:], AF.Copy, ["ub"], ["a"])
                curt, curk = A, "a"
                for k in range(g + 1):
                    sh = 2 ** k
                    dst, dk = (S1, "s1") if k % 2 == 0 else (S2, "s2")
                    tt(dst[:, 16:], curt[:, 16:], curt[:, 16 - sh:S + 16 - sh], ALU.add, [curk], [dk])
                    curt, curk = dst, dk
                stt(PL[:, ic, :], curt[:, 16:], 1.0 / wdw, A[:, 16:], ALU.mult, ALU.subtract, [curk, "a"], [("pl", ic)])
                tt(T16[:], curt[:, 16:32], INVC[:, g, :], ALU.mult, [curk], ["t16"])
                tt(PL[:, ic, 0:16], T16[:], A[:, 16:32], ALU.subtract, ["t16", "a", ("pl", ic)], [("pl", ic)])
            w, wk = W.get(pwd[(l * 4 + g) * 128:(l * 4 + g + 1) * 128, :], 512)
            w3 = w[:, 0:512].rearrange("p (i o) -> p i o", i=2)
            for oc in range(2):
                for th in range(NQ):
                    p = next_ps()
                    mm(PS[p][:], [(w3[:, ic, oc * 128:(oc + 1) * 128], PL[:, ic, th * SUB:(th + 1) * SUB]) for ic in range(2)],
                       [wk, ("pl", 0), ("pl", 1)], [("ps", p)])
                    r = yi % 2
                    yi += 1
                    act(YST[r][:], PS[p][:], AF.Copy, [("ps", p)], [("yst", r)], scale=PSC[:, l, g * 2 + oc:g * 2 + oc + 1])
                    dma(YPS[:, g * 2 + oc, th * SUB:(th + 1) * SUB], YST[r][:], [("yst", r)], (), f"yst{r}")
        P.barrier()

    def m3_stage(l):
        off = cur[0]
        limit[0] = GS_OFF
        GP = alloc("gp", [128, S], F32)
        R1 = alloc("r1", [128, S], F32)
        R2 = alloc("r2", [128, S], F32)
        MID = alloc("mid", [128, S], BF16)
        cur[0] = off
        src, sk, dst, dk = GS, "gs", GP, "gp"
        s = 1
        while s < S:
            tt(dst[0:72, s:], src[0:72, s:], src[0:72, 0:S - s], ALU.add, [sk], [dk])
            cp(dst[0:72, 0:s], src[0:72, 0:s], [sk, dk], [dk])
            src, sk, dst, dk = dst, dk, src, sk
            s *= 2
        G, gk = src, [sk]
        memset(FR[:], 0.0, ["fr"])
        cp(FR[0:72, :], G[0:72, :], gk + ["fr"], ["fr"])
        tt(R1[32:40, :], G[32:40, :], FR[32:40, :], ALU.subtract, gk + ["fr"], ["r1"])
        tt(R1[64:72, :], G[64:72, :], FR[64:72, :], ALU.subtract, gk + ["fr"], ["r1b"])
        cp(MID[32:40, :], R1[32:40, :], ["r1"], ["mid"])
        cp(MID[64:72, :], R1[64:72, :], ["r1b"], ["midb"])
        tt(R2[64:72, :], R1[64:72, :], MID[64:72, :], ALU.subtract, ["r1b", "midb"], ["r2"])
        cp(FR[32:40, :], MID[32:40, :], ["mid", "fr", "r1", "r1b"], ["fr"])
        cp(FR[64:72, :], R2[64:72, :], ["r2", "fr"], ["fr"])
        p = next_ps()
        for kb in range(NB):
            mm(PS[p][:, kb * 8:(kb + 1) * 8], [(G[0:8, kb * 128:(kb + 1) * 128], I8[0:8, 0:8])], gk, [("ps", p)])
        act(GTT[:], PS[p][:, 0:128], AF.Copy, [("ps", p)], ["gtt"])
        P.barrier()
        off = cur[0]
        FQHs = [alloc(f"fqh{i}", [128, S], BF16) for i in range(2)]
        FKHs = [alloc(f"fkh{i}", [128, S], BF16) for i in range(2)]
        FVHs = [alloc(f"fvh{i}", [128, NB, 128], BF16) for i in range(2)]
        PTL = [alloc(f"ptl{i}", [128, SUB], BF16) for i in range(3)]
        RDEN = alloc("rden", [128, SUB], F32)
        YST = [alloc(f"yst{i}", [128, SUB], BF16) for i in range(2)]
        cur[0] = off
        pi = 0
        yi = 0

        def head_loads(h):
            hb = h % 2
            dma(FQHs[hb][:], FQS[:, h, :], (), [("fqh", hb), "hchain"], "hld0")
            dma(FKHs[hb][:], FKS[:, h, :], (), [("fkh", hb), "hchain"], "hld0")
            dma(FVHs[hb][:], FVS[:, :, h * 128:(h + 1) * 128], (), [("fvh", hb), "hchain"], "hld0")

        head_loads(0)
        head_loads(1)
        its = []
        for h in range(H):
            for qg in range(NQ):
                nkb = 4 * qg + 4
                for kb in range(nkb):
                    its.append((h, qg, kb, nkb))
        state = {"pi": 0, "yi": 0}

        def emit_qk(it):
            h, qg, kb, nkb = it
            hb = h % 2
            FQH, FKH = FQHs[hb], FKHs[hb]
            c0 = max(0, kb * 128 - qg * SUB)
            n = SUB - c0
            qs = slice(qg * SUB + c0, (qg + 1) * SUB)
            diag = kb >= 4 * qg
            psn = state["pi"] % 2
            r = state["pi"] % 3
            state["pi"] += 1

            def fn(e, psn=psn, kb=kb, qs=qs, n=n, diag=diag, h=h, FKH=FKH, FQH=FQH):
                e.matmul(PS[psn][:, 0:n], FKH[:, kb * 128:(kb + 1) * 128], FQH[:, qs], start=True, stop=False)
                if diag:
                    e.matmul(PS[psn][:, 0:128], IDENT, NEGM, start=False, stop=False)
                return e.matmul(PS[psn][:, 0:n], SEL[0:72, h, :], FR[0:72, qs], start=False, stop=True)
            P.op("pe", fn, [("fqh", hb), ("fkh", hb), "fr"], [("ps", psn)])
            act(PTL[r][:, 0:n], PS[psn][:, 0:n], AF.Exp, [("ps", psn), "gtt"], [("ptl", r)],
                bias=GTT[:, kb * 8 + h:kb * 8 + h + 1], scale=1.0)
            return (it, r, c0, n)

        def emit_pv(pend):
            (h, qg, kb, nkb), r, c0, n = pend
            hb = h % 2
            FVH = FVHs[hb]
            po, pd = (2, 3) if (h * NQ + qg) % 2 == 0 else (4, 5)

            def fn2(e, po=po, pd=pd, kb=kb, c0=c0, n=n, r=r, nkb=nkb, FVH=FVH):
                e.matmul(PS[po][:, c0:SUB], FVH[:, kb, :], PTL[r][:, 0:n], start=(kb == 0), stop=(kb == nkb - 1))
                return e.matmul(PS[pd][:, c0:SUB], ONESB, PTL[r][:, 0:n], start=(kb == 0), stop=(kb == nkb - 1))
            P.op("pe", fn2, [("ptl", r), ("fvh", hb)], [("ps", po), ("ps", pd)])
            if kb == nkb - 1:
                recip(RDEN[:], PS[pd][:], [("ps", pd)], ["rden"])
                ry = state["yi"] % 2
                state["yi"] += 1
                tt(YST[ry][:], PS[po][:], RDEN[:], ALU.mult, [("ps", po), "rden"], [("yst", ry)])
                dma(YFS[:, h, qg * SUB:(qg + 1) * SUB], YST[ry][:], [("yst", ry)], (), f"yst{ry}")
                if qg == NQ - 1 and h + 2 < H:
                    head_loads(h + 2)

        pend = None
        for it in its:
            nxt = emit_qk(it)
            if pend is not None:
                emit_pv(pend)
            pend = nxt
        emit_pv(pend)
        P.barrier()

    def m3r_stage(l):
        off = cur[0]
        limit[0] = top
        RQH = alloc("rqh", [128, NB, 128], BF16)
        RKH = alloc("rkh", [128, NB, 128], BF16)
        RVH = alloc("rvh", [128, NB, 128], BF16)
        RGH = alloc("rgh", [128, S], BF16)
        RQT = alloc("rqt", [128, S], BF16)
        RQXT = alloc("rqxt", [128, S], BF16)
        RKT = alloc("rkt", [128, S], BF16)
        SDEC = alloc("sdec", [128, NB, 128], BF16)
        STB = alloc("stb", [128, NB, 128], BF16)
        STATE = alloc("state", [128, 128], F32)
        OF = alloc("of", [128, SUB], F32)
        OB = alloc("ob", [128, SUB], BF16)
        OSQ = alloc("osq", [128, SUB], BF16)
        MN = alloc("mn", [128, SUB], F32)
        T1 = alloc("t1", [128, SUB], F32)
        VAR = alloc("var", [128, SUB], F32)
        CC = alloc("cc", [128, SUB], F32)
        YST = [alloc(f"yst{i}", [128, SUB], BF16) for i in range(2)]
        cur[0] = off
        yi = 0
        RLV = int(os.environ.get("MK_R", "9"))
        XV = int(os.environ.get("MK_X", "0"))
        for h in range(H):
            cd = float(np.exp(128.0 * log_gamma(h)))
            hs = slice(h * 128, (h + 1) * 128)
            dma(RQH[:], RQS[:, :, hs], (), ["rqh", "hchain"], "hld0")
            dma(RKH[:], RKS[:, :, hs], (), ["rkh", "hchain"], "hld0")
            dma(RVH[:], RVS[:, :, hs], (), ["rvh", "hchain"], "hld0")
            dma(RGH[:], RGS[:, h, :], (), ["rgh", "hchain"], "hld0")
            if RLV < 1:
                continue
            for grp in range(4):
                gsl = slice(grp * SUB, (grp + 1) * SUB)
                a = next_pt()

                def fnq(e, a=a, grp=grp):
                    ins = None
                    for j in range(4):
                        ins = e.matmul(PS[a][:, j * 128:(j + 1) * 128], RQH[:, grp * 4 + j, :], IDENT, start=True, stop=True)
                    return ins
                P.op("pe", fnq, ["rqh"], [("ps", a)])
                if XV != 2:
                    cp(RQT[:, gsl], PS[a][:], [("ps", a)], [("rqt", grp)])
                for j in range(4 if XV != 1 else 0):
                    n = grp * 4 + j
                    tt(RQXT[:, n * 128:(n + 1) * 128], PS[a][:, j * 128:(j + 1) * 128], XIROW[:, h, :], ALU.mult,
                       [("ps", a)], [("rqxt", n)])
                if XV != 0:
                    continue
                a2 = next_pt()

                def fnk(e, a2=a2, grp=grp):
                    ins = None
                    for j in range(4):
                        ins = e.matmul(PS[a2][:, j * 128:(j + 1) * 128], RKH[:, grp * 4 + j, :], IDENT, start=True, stop=True)
                    return ins
                P.op("pe", fnk, ["rkh"], [("ps", a2)])
                act(RKT[:, gsl], PS[a2][:], AF.Copy, [("ps", a2)], [("rkt", grp)])
            if RLV < 2:
                continue
            for grp in range(4):
                p = grp % 2

                def fns(e, p=p, grp=grp):
                    ins = None
                    for j in range(4):
                        ns = slice((grp * 4 + j) * 128, (grp * 4 + j + 1) * 128)
                        ins = e.matmul(PS[p][:, j * 128:(j + 1) * 128], RKT[:, ns], RQT[:, ns], start=True, stop=True)
                    return ins
                P.op("pe", fns, [("rkt", grp), ("rqt", grp)], [("ps", p)])
                for j in range(4):
                    n = grp * 4 + j
                    tt(SDEC[:, n, :], PS[p][:, j * 128:(j + 1) * 128], DTAB[:, h, :], ALU.mult, [("ps", p)], [("sdec", n)])
            if RLV < 3:
                continue
            memset(STATE[:], 0.0, ["state"])
            kvp = [2, 3, 4, 5]
            for grp in range(4):
                p = kvp[grp]

                def fnkv(e, p=p, grp=grp):
                    ins = None
                    for j in range(4):
                        n = grp * 4 + j
                        ins = e.matmul(PS[p][:, j * 128:(j + 1) * 128], RKH[:, n, :], RVH[:, n, :], start=True, stop=True)
                    return ins
                P.op("pe", fnkv, ["rkh", "rvh"], [("ps", p)])
            for n in range(NB):
                cp(STB[:, n, :], STATE[:], ["state"], [("stb", n)])
                p, j = kvp[n // 4], n % 4
                stt(STATE[:], STATE[:], cd, PS[p][:, j * 128:(j + 1) * 128], ALU.mult, ALU.add,
                    ["state", ("ps", p)], ["state"])
            if RLV < 4:
                continue
            for grp in range(4):
                gsl = slice(grp * SUB, (grp + 1) * SUB)
                po = grp % 2

                def fno(e, po=po, grp=grp):
                    ins = None
                    for j in range(4):
                        n = grp * 4 + j
                        ns = slice(n * 128, (n + 1) * 128)
                        e.matmul(PS[po][:, j * 128:(j + 1) * 128], RVH[:, n, :], SDEC[:, n, :], start=True, stop=False)
                        ins = e.matmul(PS[po][:, j * 128:(j + 1) * 128], STB[:, n, :], RQXT[:, ns], start=False, stop=True)
                    return ins
                P.op("pe", fno, ["rvh"] + [(k, grp * 4 + j) for j in range(4) for k in ("sdec", "stb", "rqxt")], [("ps", po)])
                pok = [("ps", po)]
                act(OF[:], PS[po][:], AF.Copy, pok, ["of"])
                act(OB[:], PS[po][:], AF.Copy, pok, ["ob"])
                act(OSQ[:], PS[po][:], AF.Square, pok, ["osq"])
                if RLV < 5:
                    continue
                pm, pq = 1 - (grp % 2), kvp[grp]
                mm(PS[pm][:], [(ONES128, OB[:])], ["ob"], [("ps", pm)])
                mm(PS[pq][:], [(ONES128, OSQ[:])], ["osq"], [("ps", pq)])
                act(MN[:], PS[pm][:], AF.Copy, [("ps", pm)], ["mn"])
                tt(T1[:], MN[:], MN[:], ALU.mult, ["mn"], ["t1"])
                stt(VAR[:], T1[:], -1.0, PS[pq][:], ALU.mult, ALU.add, ["t1", ("ps", pq)], ["var"])
                ts(VAR[:], VAR[:], 0.0, ALU.max, ["var"], ["var"])
                act(T1[:], VAR[:], AF.Sqrt, ["var", "t1"], ["t1"], bias=EPS_GN[:, 0:1], scale=1.0)
                recip(VAR[:], T1[:], ["t1", "var"], ["var"])
                tt(CC[:], OF[:], MN[:], ALU.subtract, ["of", "mn"], ["cc"])
                tt(CC[:], CC[:], VAR[:], ALU.mult, ["cc", "var"], ["cc"])
                r = yi % 2
                yi += 1
                tt(YST[r][:], CC[:], RGH[:, gsl], ALU.mult, ["cc", "rgh"], [("yst", r)])
                dma(YRS[:, h, gsl], YST[r][:], [("yst", r)], (), f"yst{r}")
        P.barrier()

    def m4_stage(l):
        off = cur[0]
        limit[0] = top
        MG = alloc("mg", [128, KC, TT], BF16)
        SG = [alloc(f"sg{i}", [128, SUB], F32) for i in range(3)]
        MACC = [alloc(f"macc{i}", [128, SUB], F32) for i in range(2)]
        TMP = [alloc(f"tmp{i}", [128, SUB], F32) for i in range(2)]
        cur[0] = off
        YT = [alloc(f"yt{i}", [128, 8, TT], BF16, at=offs["xt0"] + i * 8 * TT * 2) for i in range(3)]
        ysrc = [YPS, YFS, YRS]
        for t in range(NT):
            tsl = slice(t * TT, (t + 1) * TT)
            for sub in range(NS):
                c0 = t * TT + sub * SUB
                dma(XN[:, :, ssl(sub)], XNS[:, :, c0:c0 + SUB], (), xnk(sub), f"xnld{sub}")
                for i in range(3):
                    dma(YT[i][:, :, ssl(sub)], ysrc[i][:, :, c0:c0 + SUB], (), [("yt", i, sub)], f"yld{i}{sub}")
            for dc in range(KC):
                r0 = (l * 16 + dc) * 128
                wa, wak = W.get(wgad[r0:r0 + 128, :], 4096)
                wb, wbk = W.get(wgbd[r0:r0 + 128, :], 2048, group=True)
                wr, wrk = W.get(wbrd[r0:r0 + 128, :], 3072, group=True)
                wa3 = wa[:, 0:4096].rearrange("p (k c) -> p k c", k=KC)
                wb3 = wb[:, 0:2048].rearrange("p (k c) -> p k c", k=KC)
                wr4 = wr[:, 0:3072].rearrange("p (b c j) -> p b c j", b=3, c=8)
                for br in range(3):
                    for sub in range(NS):
                        pg, pb = next_ps(), next_ps()
                        if br < 2:
                            mm(PS[pg][:], [(wa3[:, kc, br * 128:(br + 1) * 128], XN[:, kc, ssl(sub)]) for kc in range(KC)],
                               [wak] + xnk(sub), [("ps", pg)])
                        else:
                            mm(PS[pg][:], [(wb3[:, kc, :], XN[:, kc, ssl(sub)]) for kc in range(KC)], [wbk] + xnk(sub), [("ps", pg)])
                        mm(PS[pb][:], [(wr4[:, br, c, :], YT[br][:, c, ssl(sub)]) for c in range(8)], [wrk, ("yt", br, sub)], [("ps", pb)])
                        act(SG[br][:], PS[pg][:], AF.Sigmoid, [("ps", pg)], [("sg", br)])
                        if br == 0:
                            tt(MACC[sub][:], SG[0][:], PS[pb][:], ALU.mult, [("sg", 0), ("ps", pb)], [("macc", sub)])
                        elif br == 1:
                            tt(TMP[0][:], SG[1][:], PS[pb][:], ALU.mult, [("sg", 1), ("ps", pb)], [("tmp", 0)])
                            tt(MACC[sub][:], MACC[sub][:], TMP[0][:], ALU.add, [("macc", sub), ("tmp", 0)], [("macc", sub)])
                        else:
                            tt(TMP[1][:], SG[2][:], PS[pb][:], ALU.mult, [("sg", 2), ("ps", pb)], [("tmp", 1)])
                            tt(MG[:, dc, ssl(sub)], MACC[sub][:], TMP[1][:], ALU.add, [("macc", sub), ("tmp", 1)], [("mg", dc, sub)])
            P.barrier()
            load_x(XS, t)
            for dp in range(8):
                r0 = (l * 8 + dp) * 128
                w, wk = W.get(woutd[r0:r0 + 128, :], 4096)
                w3 = w[:, 0:4096].rearrange("p (k c) -> p k c", k=KC)
                for j in range(2):
                    dc = dp * 2 + j
                    for sub in range(NS):
                        p = next_ps()
                        mm(PS[p][:], [(w3[:, kc, j * 128:(j + 1) * 128], MG[:, kc, ssl(sub)]) for kc in range(KC)],
                           [wk] + [("mg", kc, sub) for kc in range(KC)], [("ps", p)])
                        tt(XT[0][:, dc, ssl(sub)], PS[p][:], XT[0][:, dc, ssl(sub)], ALU.add,
                           [("ps", p), ("xt", 0, dc, sub)], [("xt", 0, dc, sub)])
            store_x(XS, t)
            P.barrier()

    GS = alloc("gs", [128, S], F32, at=GS_OFF)
    FR = alloc("fr", [128, S], BF16, at=GS_OFF + 8192)
    GTT = alloc("gtt", [128, 128], F32)
    ONE_C = alloc("onec", [128, 2], F32)
    EPS_GN = alloc("epsg", [128, 2], F32)
    ov_base = cur[0]

    def trace():
        W.cons = 0
        W.safe = 0
        psi[0] = 0
        pti[0] = 0
        dma(CF[:], cfd[:, :], (), ["cf"], "cf")
        dma(CB[:], cbd[:, :], (), ["cb"], "cb")
        ts(NFB[:], FBR, -1.0, ALU.mult, ["cf"], ["nfb"])
        memset(EPS_RMS[:], RMS_EPS, ["epsr"])
        memset(EPS_GN[:], GN_EPS, ["epsg"])
        memset(ONE_C[:], 1.0, ["onec"])
        P.barrier()
        st = os.environ.get("MK_STAGES", "f0,m1,m2,m3,m3r,m4,f1").split(",")
        for l in range(L):
            if "f0" in st:
                ffn_stage(l, 0, xin if l == 0 else XS, XS, False)
            if "m1" in st:
                m1_stage(l)
            if "m2" in st:
                m2_stage(l)
            if "m3" in st:
                m3_stage(l)
            if "m3r" in st:
                m3r_stage(l)
            if "m4" in st:
                m4_stage(l)
            if "f1" in st:
                ffn_stage(l, 1, XS, xout if l == L - 1 else XS, l == L - 1)

    P.dry = True
    trace()
    P.dry = False
    trace()
    P.barrier()
    P.emit()
    return nc


def _consts(L):
    NG = L * 3 + 1
    lg = np.array([np.log1p(-np.power(2.0, -5.0 - h)) for h in range(H)], np.float64)
    p = np.arange(128, dtype=np.float64)
    zeta = (128.0 ** -0.5) * np.exp((127.0 - p)[:, None] * lg[None, :])
    i8 = np.zeros((128, 8)); i8[:8, :8] = np.eye(8)
    q = np.arange(128, dtype=np.float64)
    dtab = np.zeros((128, 8, 128))
    for h in range(H):
        m = (p[:, None] <= q[None, :])
        dtab[:, h, :] = np.where(m, np.exp((q[None, :] - 127.0) * lg[h]), 0.0)
    xirow = np.zeros((128, 8, 128))
    for h in range(H):
        xirow[:, h, :] = np.exp((q + 1.0) * lg[h])[None, :]
    invc = np.zeros((128, 4, 16))
    for g in range(4):
        w = 2 ** (g + 1)
        invc[:, g, :] = 1.0 / np.minimum(np.arange(16) + 1, w)[None, :]
    tail = np.concatenate([zeta, i8, dtab.reshape(128, -1), xirow.reshape(128, -1), invc.reshape(128, -1)], axis=1)
    cb = np.zeros((128, 4 * 128 + 8 * 128), np.float32)
    cb[:, 0:128] = 1.0
    cb[:, 128:256] = 1.0 / 128.0
    cb[:, 256:384] = np.eye(128)
    cb[:, 384:512] = np.where(p[:, None] > q[None, :], -30000.0, 0.0)
    sel = np.zeros((128, 8, 128), np.float32)
    for h in range(H):
        for r in (h, 32 + h, 64 + h):
            sel[r, h, :] = -1.0
    cb[:, 512:] = sel.reshape(128, -1)
    half = 64
    inv_freq = (np.float32(10000.0) ** (-np.arange(half, dtype=np.float32) / np.float32(half))).astype(np.float32)
    pos = np.arange(S, dtype=np.float32)
    ang = (pos[:, None] * inv_freq[None, :]).astype(np.float32).astype(np.float64)
    cos, sin = np.cos(ang), np.sin(ang)
    cos2 = np.concatenate([cos, cos], axis=1)
    sins = np.concatenate([-sin, sin], axis=1)
    cosr = np.tile(cos2.reshape(NB, 128, 1, 128), (1, 1, 2, 1)).transpose(1, 0, 2, 3).reshape(128, NB, 256)
    sinr = np.tile(sins.reshape(NB, 128, 1, 128), (1, 1, 2, 1)).transpose(1, 0, 2, 3).reshape(128, NB, 256)
    return tail.astype(np.float32), cb.astype(ml_dtypes.bfloat16), np.ascontiguousarray(cosr, np.float32), np.ascontiguousarray(sinr, np.float32)


def _fm(v):
    return v.reshape(v.shape[:-1] + (KC, 128))


def _layout_weights(inp, ls):
    L = len(ls)
    w13 = np.empty((L, 2, FC, 128, KC, 2, 128), np.float32)
    w2 = np.empty((L, 2, 16, 2, 128, 22, 128), np.float32)
    for i, l in enumerate(ls):
        for wi, (a, b) in enumerate((("ffn1_w13", "ffn1_w2"), ("ffn2_w13", "ffn2_w2"))):
            w13[i, wi] = inp[a][l].reshape(KC, 128, 2, FC, 128).transpose(3, 1, 0, 2, 4)
            w2[i, wi] = inp[b][l].reshape(2, 22, 128, 16, 128).transpose(3, 0, 2, 1, 4)
    win = np.empty((L, 32, 128, KC, 256), np.float32)
    wfl = np.zeros((L, 128, KC, 72), np.float32)
    wga = np.empty((L, 16, 128, KC, 2, 128), np.float32)
    wgb = np.empty((L, 16, 128, KC, 128), np.float32)
    wbr = np.empty((L, 16, 128, 3, 8, 128), np.float32)
    wout = np.empty((L, 8, 128, KC, 256), np.float32)
    pw = np.empty((L, 4, 128, 2, 256), np.float32)
    order = [OFF_POOL, OFF_FQ, OFF_FK, OFF_RG, OFF_FV, OFF_RV, OFF_RQ, OFF_RK]
    for i, l in enumerate(ls):
        wi_ = inp["w_in"][l]
        for fi, off in enumerate(order):
            win[i, fi * 4:(fi + 1) * 4] = wi_[:, off:off + 1024].reshape(KC, 128, 4, 256).transpose(2, 1, 0, 3)
        fl = wi_[:, OFF_FL:OFF_FL + 8].reshape(KC, 128, 8).transpose(1, 0, 2)
        for r in (0, 32, 64):
            wfl[i, :, :, r:r + 8] = fl
        gts = wi_[:, OFF_GATE:OFF_GATE + 3 * D]
        wga[i] = gts[:, 0:2 * D].reshape(KC, 128, 2, 16, 128).transpose(3, 1, 0, 2, 4)
        wgb[i] = gts[:, 2 * D:3 * D].reshape(KC, 128, 16, 128).transpose(2, 1, 0, 3)
        br = np.stack([inp["w_branch_pool"][l], inp["w_branch_fox"][l], inp["w_branch_ret"][l]], 0)
        wbr[i] = br.reshape(3, 8, 128, 16, 128).transpose(3, 2, 0, 1, 4)
        wout[i] = inp["w_out"][l].reshape(KC, 128, 8, 256).transpose(2, 1, 0, 3)
        pw[i] = inp["pool_w"][l].reshape(4, 2, 128, 256).transpose(0, 2, 1, 3)
    NG = L * 3 + 1
    gains = np.zeros((NG, D), np.float32)
    for i, l in enumerate(ls):
        gains[i * 3 + 0] = inp["ffn1_norm"][l]
        gains[i * 3 + 1] = inp["mix_norm"][l]
        gains[i * 3 + 2] = inp["ffn2_norm"][l]
    gains[L * 3] = inp["final_norm"]
    gain_fm = gains.reshape(NG, KC, 128).transpose(2, 0, 1).reshape(128, NG * KC)
    psc = np.stack([inp["pool_scale"][l] for l in ls], 0).reshape(L, 8, 128).transpose(2, 0, 1).reshape(128, L * 8)
    fbr = np.zeros((128, L), np.float32)
    for i, l in enumerate(ls):
        for r in (0, 32, 64):
            fbr[r:r + 8, i] = inp["forget_bias"][l]
    tail, cb, cosr, sinr = _consts(L)
    cf = np.ascontiguousarray(np.concatenate([gain_fm, psc, fbr, tail], axis=1), np.float32)
    return {
        "w13": w13.reshape(-1, 4096), "w2": w2.reshape(-1, 2816), "win": win.reshape(-1, 4096),
        "wfl": wfl.reshape(-1, KC * 72), "wga": wga.reshape(-1, 4096), "wgb": wgb.reshape(-1, 2048),
        "wbr": wbr.reshape(-1, 3072), "wout": wout.reshape(-1, 4096), "pw": pw.reshape(-1, 512),
        "cf": cf, "cb": cb, "cosr": cosr, "sinr": sinr,
    }


_NC_CACHE = {}


def _get_nc(nl):
    if nl not in _NC_CACHE:
        _NC_CACHE[nl] = build(nl)
    return _NC_CACHE[nl]


LAYERS_PER_LAUNCH = int(os.environ.get("MK_LPL", "4"))
N_LAYERS = int(os.environ.get("MK_NL", str(DEPTH)))


def kernel(**inp):
    inp = {k: np.asarray(v) for k, v in inp.items()}
    B = inp["x"].shape[0]
    xs = [np.ascontiguousarray(inp["x"][b].T.reshape(KC, 128, S).transpose(1, 0, 2)) for b in range(B)]
    ys = None
    l0 = 0
    while l0 < N_LAYERS:
        ls = list(range(l0, min(N_LAYERS, l0 + LAYERS_PER_LAUNCH)))
        nc = _get_nc(len(ls))
        wl = _layout_weights(inp, ls)
        in_maps = [dict(wl, xin=xs[b]) for b in range(B)]
        res = run_bass_kernel_spmd(nc, in_maps, core_ids=list(range(B)))
        xs = [np.ascontiguousarray(res.results[b]["xout"]) for b in range(B)]
        if os.environ.get("MK_DEBUG") == "1":
            global LAST_RES
            LAST_RES = res.results
        ys = [res.results[b]["yout"] for b in range(B)]
        l0 += len(ls)
    out = np.stack([y.transpose(1, 0, 2).reshape(D, S).T for y in ys], 0)
    return np.ascontiguousarray(out, np.float32)
```
